# Optimizing a Trainium2 kernel written in Bass

```python
import math
import jax, jax.numpy as jnp
from jax import lax
import numpy as np

D_MODEL = 1024
BATCH = 8
SEQ = 8192
DEPTH = 2

GRID_W = 64
CTX_LEN = 256
N_MIXERS = 2
N_HG_LAYERS = (DEPTH + N_MIXERS - 1) // N_MIXERS
N_DA_LAYERS = DEPTH // N_MIXERS
HG_HEADS = D_MODEL // 128
HG_DK = 128
HG_DV = D_MODEL // HG_HEADS
HG_CHUNK = 64
DA_HEADS = 8
DA_DH = D_MODEL // DA_HEADS // 2
DA_DV = 2 * DA_DH
Q_BLOCK = 128
ROPE_BASE = 10000.0
N_EXPERTS = 32
TOP_K = 4
D_EXPERT = D_MODEL
SWIGLU_LIMIT = 7.0
SWIGLU_ALPHA = 1.702
MOE_BLOCK = 128
DN_ALPHA = (2 * DEPTH) ** 0.25
DN_BETA = (8 * DEPTH) ** -0.25
LN_EPS = 1e-5
RMS_EPS = 1e-6

kernel_name = 'hybrid_hgrn2_diffattn_moe_dit'


def layer_norm(x, g, b):
    xf = x.astype(jnp.float32)
    mu = jnp.mean(xf, axis=-1, keepdims=True)
    var = jnp.mean(jnp.square(xf - mu), axis=-1, keepdims=True)
    y = (xf - mu) * lax.rsqrt(var + LN_EPS) * g.astype(jnp.float32) + b.astype(jnp.float32)
    return y.astype(x.dtype)


def rms_norm(x, g):
    xf = x.astype(jnp.float32)
    return xf * lax.rsqrt(jnp.mean(xf * xf, axis=-1, keepdims=True) + RMS_EPS) * g.astype(jnp.float32)


def rope_2d(t):
    L = t.shape[1]
    rows_n = L // GRID_W
    row = jnp.repeat(jnp.arange(rows_n), GRID_W)
    col = jnp.tile(jnp.arange(GRID_W), rows_n)
    half = t.shape[-1] // 2
    n_freq = half // 2
    inv = ROPE_BASE ** (-jnp.arange(n_freq, dtype=jnp.float32) / n_freq)

    def rot(u, pos):
        ang = pos.astype(jnp.float32)[:, None] * inv[None, :]
        cos = jnp.cos(ang)[None, :, None, :]
        sin = jnp.sin(ang)[None, :, None, :]
        u1, u2 = jnp.split(u, 2, axis=-1)
        return jnp.concatenate([u1 * cos - u2 * sin, u1 * sin + u2 * cos], axis=-1)

    tf = t.astype(jnp.float32)
    out = jnp.concatenate([rot(tf[..., :half], row), rot(tf[..., half:], col)], axis=-1)
    return out.astype(t.dtype)


def gla_chunkwise(q, k, v, logf, s0):
    bsz, L, H, _ = q.shape
    DV = v.shape[-1]
    n = L // HG_CHUNK

    def chunks(t):
        return t.reshape(bsz, n, HG_CHUNK, H, t.shape[-1]).transpose(1, 0, 3, 2, 4)

    qc, kc, vc, gc = chunks(q), chunks(k), chunks(v), chunks(logf)
    b = jnp.cumsum(gc, axis=3)
    b_end = b[:, :, :, -1:, :]
    q_dec = qc * jnp.exp(b)
    k_inv = kc * jnp.exp(-b)
    k_end = kc * jnp.exp(b_end - b)
    tri = jnp.tril(jnp.ones((HG_CHUNK, HG_CHUNK), dtype=bool))
    a = jnp.where(tri, jnp.einsum('nbhtk,nbhsk->nbhts', q_dec, k_inv), 0.0)
    o_intra = jnp.einsum('nbhts,nbhsv->nbhtv', a, vc)

    def step(state, xs):
        qn, kn, vn, dn = xs
        o = jnp.einsum('bhtk,bhkv->bhtv', qn, state)
        state = state * jnp.exp(dn)[..., None] + jnp.einsum('bhsk,bhsv->bhkv', kn, vn)
        return state, o

    s_final, o_inter = lax.scan(step, s0, (q_dec, k_end, vc, b_end[:, :, :, 0, :]))
    o = (o_intra + o_inter).transpose(1, 0, 3, 2, 4).reshape(bsz, L, H, DV)
    return o, s_final


def hgrn2_mixer(h_lat, h_ctx, w_in, lb_tab, norm_g, w_out, layer, need_ctx):
    lb = jnp.cumsum(jax.nn.softmax(lb_tab.astype(jnp.float32), axis=1), axis=1)[:, layer]
    lb = lb.reshape(2, HG_HEADS, HG_DK)

    def heads(t):
        return t.reshape(t.shape[0], t.shape[1], HG_HEADS, -1).astype(jnp.float32)

    def project(h):
        q, i, g, zf, zb = jnp.split(h @ w_in, 5, axis=-1)
        return heads(q), heads(i), g, (heads(zf), heads(zb))

    def scan_dir(q, i, z, lbd, s0, reverse):
        if reverse:
            q, i, z = jnp.flip(q, 1), jnp.flip(i, 1), jnp.flip(z, 1)
        f = lbd + (1.0 - lbd) * jax.nn.sigmoid(z)
        o, s = gla_chunkwise(q, 1.0 - f, i, jnp.log(f), s0)
        if reverse:
            o = jnp.flip(o, 1)
        return o, s

    def readout(o, g):
        o = rms_norm(o, norm_g.reshape(HG_HEADS, HG_DV)) * jax.nn.silu(heads(g))
        return o.reshape(o.shape[0], o.shape[1], D_MODEL).astype(w_out.dtype) @ w_out

    qc, ic, gc, zc = project(h_ctx)
    ql, il, gl, zl = project(h_lat)
    s0 = jnp.zeros((h_ctx.shape[0], HG_HEADS, HG_DK, HG_DV), jnp.float32)
    oc_f, sc_f = scan_dir(qc, ic, zc[0], lb[0], s0, False)
    oc_b, sc_b = scan_dir(qc, ic, zc[1], lb[1], s0, True)
    ol_f, _ = scan_dir(ql, il, zl[0], lb[0], sc_f, False)
    ol_b, _ = scan_dir(ql, il, zl[1], lb[1], sc_b, True)
    out_lat = readout(ol_f + ol_b, gl)
    out_ctx = readout(oc_f + oc_b, gc) if need_ctx else None
    return out_lat, out_ctx


def diff_attn_mixer(h_lat, h_ctx, w_in, lam_p, norm_g, w_out, layer, need_ctx):
    lam_init = 0.8 - 0.6 * math.exp(-0.3 * layer)
    lp = lam_p.astype(jnp.float32)
    lam = jnp.exp(jnp.sum(lp[0] * lp[1])) - jnp.exp(jnp.sum(lp[2] * lp[3])) + lam_init
    w_q, w_kv = w_in[:, :D_MODEL], w_in[:, D_MODEL:]

    def proj_q(h):
        return (h @ w_q).reshape(h.shape[0], h.shape[1], 2 * DA_HEADS, DA_DH)

    def proj_kv(h):
        k, v = jnp.split(h @ w_kv, 2, axis=-1)
        return (k.reshape(h.shape[0], h.shape[1], 2 * DA_HEADS, DA_DH),
                v.reshape(h.shape[0], h.shape[1], DA_HEADS, DA_DV))

    def maps(t):
        return t.reshape(t.shape[0], t.shape[1], DA_HEADS, 2, DA_DH)

    def attend(qb, k, v):
        s = jnp.einsum('bqhcd,bkhcd->bhcqk', qb, k).astype(jnp.float32) * (DA_DH ** -0.5)
        p = jax.nn.softmax(s, axis=-1)
        w = p[:, :, 0] - lam * p[:, :, 1]
        return jnp.einsum('bhqk,bkhv->bqhv', w.astype(v.dtype), v)

    def readout(o):
        o = rms_norm(o, norm_g) * (1.0 - lam_init)
        return o.reshape(o.shape[0], o.shape[1], D_MODEL).astype(w_out.dtype) @ w_out

    ql = rope_2d(proj_q(h_lat))
    kl, vl = proj_kv(h_lat)
    kl = rope_2d(kl)
    kc, vc = proj_kv(h_ctx)
    k_all = maps(jnp.concatenate([kl, kc], axis=1))
    v_all = jnp.concatenate([vl, vc], axis=1)
    bsz, L = ql.shape[0], ql.shape[1]
    nb = L // Q_BLOCK
    q_blocks = maps(ql).reshape(bsz, nb, Q_BLOCK, DA_HEADS, 2, DA_DH).transpose(1, 0, 2, 3, 4, 5)
    o_lat = lax.map(lambda qb: attend(qb, k_all, v_all), q_blocks)
    o_lat = o_lat.transpose(1, 0, 2, 3, 4).reshape(bsz, L, DA_HEADS, DA_DV)
    out_lat = readout(o_lat)
    out_ctx = readout(attend(maps(proj_q(h_ctx)), maps(kc), vc)) if need_ctx else None
    return out_lat, out_ctx


def moe_ffn(h, w_r, b_r, w_gu, b_gu, w_dn, b_dn):
    T, D = h.shape
    logits = (h @ w_r + b_r).astype(jnp.float32)
    top_v, top_i = lax.top_k(logits, TOP_K)
    gates = jax.nn.softmax(top_v, axis=-1)
    A = T * TOP_K
    e_flat = top_i.reshape(-1)
    order = jnp.argsort(e_flat, stable=True)
    e_sorted = e_flat[order]
    tok_sorted = order // TOP_K
    g_sorted = gates.reshape(-1)[order].astype(h.dtype)
    counts = jnp.bincount(e_flat, length=N_EXPERTS)
    padded = (counts + MOE_BLOCK - 1) // MOE_BLOCK * MOE_BLOCK
    start = jnp.cumsum(counts) - counts
    pend = jnp.cumsum(padded)
    pstart = pend - padded
    dest = pstart[e_sorted] + (jnp.arange(A) - start[e_sorted])
    n_blocks = -(-A // MOE_BLOCK) + N_EXPERTS
    buf = jnp.zeros((n_blocks * MOE_BLOCK, D), h.dtype).at[dest].set(h[tok_sorted])
    block_expert = jnp.minimum(jnp.searchsorted(pend, jnp.arange(n_blocks) * MOE_BLOCK, side='right'), N_EXPERTS - 1)

    def expert_block(args):
        xb, e = args
        gate, up = jnp.split(xb @ w_gu[e] + b_gu[e], 2, axis=-1)
        gate = jnp.minimum(gate, SWIGLU_LIMIT)
        up = jnp.clip(up, -SWIGLU_LIMIT, SWIGLU_LIMIT)
        act = (up + 1.0) * gate * jax.nn.sigmoid(SWIGLU_ALPHA * gate)
        return act @ w_dn[e] + b_dn[e]

    out = lax.map(expert_block, (buf.reshape(n_blocks, MOE_BLOCK, D), block_expert)).reshape(-1, D)
    return jax.ops.segment_sum(out[dest] * g_sorted[:, None], tok_sorted, num_segments=T)


def setup_inputs(seed: int = 0) -> dict:
    key = jax.random.key(seed)
    ks = jax.random.split(key, 22)
    D = D_MODEL

    def nrm(k, shape, scale):
        return jax.random.normal(k, shape, jnp.float32) * scale

    return {
        'x': nrm(ks[0], (BATCH, SEQ, D), 1.0),
        'c': nrm(ks[1], (BATCH, D), 1.0),
        'ctx': nrm(ks[2], (BATCH, CTX_LEN, D), 1.0),
        'c_ctx': nrm(ks[3], (D,), 1.0),
        'w_ada': nrm(ks[4], (DEPTH, D, 6 * D), 0.5 * D ** -0.5),
        'b_ada': nrm(ks[5], (DEPTH, 6 * D), 0.02),
        'ln_g': 1.0 + nrm(ks[6], (DEPTH, 2, D), 0.02),
        'ln_b': nrm(ks[7], (DEPTH, 2, D), 0.02),
        'hg_w_in': nrm(ks[8], (N_HG_LAYERS, D, 5 * D), D ** -0.5),
        'hg_lb': nrm(ks[9], (2, DEPTH + 1, HG_HEADS * HG_DK), 0.1),
        'hg_norm_g': 1.0 + nrm(ks[10], (N_HG_LAYERS, D), 0.02),
        'hg_w_out': nrm(ks[11], (N_HG_LAYERS, D, D), DN_BETA * D ** -0.5),
        'da_w_in': nrm(ks[12], (N_DA_LAYERS, D, 3 * D), D ** -0.5),
        'da_lam': nrm(ks[13], (N_DA_LAYERS, 4, DA_DH), 0.1),
        'da_norm_g': 1.0 + nrm(ks[14], (N_DA_LAYERS, DA_DV), 0.02),
        'da_w_out': nrm(ks[15], (N_DA_LAYERS, D, D), DN_BETA * D ** -0.5),
        'moe_w_router': nrm(ks[16], (DEPTH, D, N_EXPERTS), D ** -0.5),
        'moe_b_router': nrm(ks[17], (DEPTH, N_EXPERTS), 0.01),
        'moe_w_gu': nrm(ks[18], (DEPTH, N_EXPERTS, D, 2 * D_EXPERT), D ** -0.5),
        'moe_b_gu': nrm(ks[19], (DEPTH, N_EXPERTS, 2 * D_EXPERT), 0.01),
        'moe_w_dn': nrm(ks[20], (DEPTH, N_EXPERTS, D_EXPERT, D), DN_BETA * D_EXPERT ** -0.5),
        'moe_b_dn': nrm(ks[21], (DEPTH, N_EXPERTS, D), 0.01),
    }


def reference(x, c, ctx, c_ctx, w_ada, b_ada, ln_g, ln_b, hg_w_in, hg_lb, hg_norm_g, hg_w_out,
              da_w_in, da_lam, da_norm_g, da_w_out, moe_w_router, moe_b_router, moe_w_gu, moe_b_gu,
              moe_w_dn, moe_b_dn):
    bsz, L, D = x.shape
    xl, xc = x, ctx
    silu_c = jax.nn.silu(c)
    silu_cc = jax.nn.silu(c_ctx)
    for l in range(DEPTH):
        last = l == DEPTH - 1
        j = l // N_MIXERS
        mod_l = (silu_c @ w_ada[l] + b_ada[l])[:, None, :]
        mod_c = (silu_cc @ w_ada[l] + b_ada[l])[None, None, :]
        sh1, sc1, g1, sh2, sc2, g2 = jnp.split(mod_l, 6, axis=-1)
        csh1, csc1, cg1, csh2, csc2, cg2 = jnp.split(mod_c, 6, axis=-1)
        hl = xl * (1.0 + sc1) + sh1
        hc = xc * (1.0 + csc1) + csh1
        if l % N_MIXERS == 0:
            ml, mc = hgrn2_mixer(hl, hc, hg_w_in[j], hg_lb, hg_norm_g[j], hg_w_out[j], l, not last)
        else:
            ml, mc = diff_attn_mixer(hl, hc, da_w_in[j], da_lam[j], da_norm_g[j], da_w_out[j], l, not last)
        xl = layer_norm(DN_ALPHA * xl + g1 * ml, ln_g[l, 0], ln_b[l, 0])
        hl2 = (xl * (1.0 + sc2) + sh2).reshape(-1, D)
        if last:
            f = moe_ffn(hl2, moe_w_router[l], moe_b_router[l], moe_w_gu[l], moe_b_gu[l], moe_w_dn[l], moe_b_dn[l])
            xl = layer_norm(DN_ALPHA * xl + g2 * f.reshape(bsz, L, D), ln_g[l, 1], ln_b[l, 1])
        else:
            xc = layer_norm(DN_ALPHA * xc + cg1 * mc, ln_g[l, 0], ln_b[l, 0])
            hc2 = (xc * (1.0 + csc2) + csh2).reshape(-1, D)
            f = moe_ffn(jnp.concatenate([hl2, hc2], axis=0), moe_w_router[l], moe_b_router[l],
                        moe_w_gu[l], moe_b_gu[l], moe_w_dn[l], moe_b_dn[l])
            fl = f[:bsz * L].reshape(bsz, L, D)
            fc = f[bsz * L:].reshape(xc.shape)
            xl = layer_norm(DN_ALPHA * xl + g2 * fl, ln_g[l, 1], ln_b[l, 1])
            xc = layer_norm(DN_ALPHA * xc + cg2 * fc, ln_g[l, 1], ln_b[l, 1])
    return xl
```

```python
import numpy as np
from contextlib import ExitStack
import concourse.bass as bass
import concourse.mybir as mybir
from concourse.bass_utils import run_bass_kernel_spmd

F32 = mybir.dt.float32
BF16 = mybir.dt.bfloat16
U8 = mybir.dt.uint8
AF = mybir.ActivationFunctionType
ALU = mybir.AluOpType
AX = mybir.AxisListType
_ISZ = {F32: 4, BF16: 2}

D = 1024
T_CTX = 256
T_LAT = 8192
T_ALL = T_CTX + T_LAT
NT = T_ALL // 128
NTL = T_LAT // 128
NE = 32
ALPHA = 4.0 ** 0.25
LN_EPS = 1e-5
RMS_EPS = 1e-6
LAM_INIT1 = 0.8 - 0.6 * float(np.exp(-0.3 * 1))
N_CORES = 8
import os as _os
_KDEBUG = bool(_os.environ.get("KDEBUG"))


class Buf:
    __slots__ = ("w", "rd", "rdd", "wx")

    def __init__(self):
        self.w = None
        self.wx = []
        self.rd = {}
        self.rdd = []


class Op:
    __slots__ = ("eng", "fn", "waits", "sig", "idx", "dma", "slot", "semval", "sigval", "tb", "force", "small")


class Sched:
    ENGS = ("pe", "act", "dve", "pool", "sp")
    K = 8

    def __init__(self):
        self.ops = {e: [] for e in self.ENGS}
        self.waited = {e: {} for e in self.ENGS}
        self.dq = {e: [] for e in self.ENGS}
        self.pending = {e: [] for e in self.ENGS}
        self.lastc = {e: None for e in self.ENGS}

    def _dep(self, c, p):
        if p is None:
            return
        w = self.waited[c.eng]
        if p.dma:
            key = ("d", p.eng, p.slot)
            if w.get(key, 0) >= p.semval:
                return
            w[key] = p.semval
            c.waits.append(p)
        else:
            if p.eng == c.eng and not c.dma and not c.force and not p.small:
                return
            key = ("c", p.eng)
            if w.get(key, -1) >= p.idx:
                return
            w[key] = p.idx
            p.sig = True
            c.waits.append(p)

    def add(self, eng, fn, R=(), W=(), dma=False, force=False, small=False):
        op = Op()
        op.force = force
        op.small = small
        op.eng = eng
        op.fn = fn
        op.waits = []
        op.sig = False
        op.dma = dma
        op.slot = 0
        op.semval = 0
        op.sigval = 0
        op.idx = len(self.ops[eng])
        op.tb = None
        if _KDEBUG:
            import traceback
            op.tb = traceback.extract_stack(limit=5)
        if self.pending[eng]:
            for p in self.pending[eng]:
                self._dep(op, p)
            self.pending[eng] = []
        for b in R:
            self._dep(op, b.w)
            for p in b.wx:
                self._dep(op, p)
        for b in W:
            self._dep(op, b.w)
            for p in b.wx:
                self._dep(op, p)
            for p in b.rd.values():
                self._dep(op, p)
            for p in b.rdd:
                self._dep(op, p)
        if dma:
            q = self.dq[eng]
            n = len(q)
            op.slot = n % self.K
            op.semval = 16 * (n // self.K + 1)
            if n >= self.K:
                self._dep(op, q[n - self.K])
            q.append(op)
        for b in R:
            if dma:
                b.rdd.append(op)
            else:
                b.rd[eng] = op
        for b in W:
            if dma and b.w is not None and b.w.dma and not b.rd and not b.rdd:
                b.wx.append(b.w)
            else:
                b.wx = []
            b.w = op
            b.rd = {}
            b.rdd = []
        self.ops[eng].append(op)
        if not dma:
            self.lastc[eng] = op
        return op

    def barrier(self):
        lst = [self.lastc[e] for e in self.ENGS if self.lastc[e] is not None]
        for e in self.ENGS:
            lst += self.dq[e][-self.K:]
        for e in self.ENGS:
            self.pending[e] = list(lst)

    def finish(self):
        self.barrier()
        for e in self.ENGS:
            self.add(e, None)

    def emit(self, nc):
        for e in self.ENGS:
            cnt = 0
            for op in self.ops[e]:
                if op.sig:
                    cnt += 1
                    op.sigval = cnt
        with ExitStack() as st:
            csem = {e: st.enter_context(nc.semaphore("c_" + e)) for e in self.ENGS}
            dsem = {}
            for e in self.ENGS:
                if self.dq[e]:
                    for k in range(self.K):
                        dsem[(e, k)] = st.enter_context(nc.semaphore("d_%s%d" % (e, k)))
            block = st.enter_context(nc.Block())

            def run(e):
                def body(eng):
                    for op in self.ops[e]:
                        for p in op.waits:
                            if p.dma:
                                eng.wait_ge(dsem[(p.eng, p.slot)], p.semval)
                            else:
                                eng.wait_ge(csem[p.eng], p.sigval)
                        if op.fn is None:
                            continue
                        try:
                            ins = op.fn(eng)
                        except Exception:
                            if op.tb is not None:
                                print("FAILED OP from:", [(f.lineno, f.line) for f in op.tb[:-1]])
                            raise
                        if op.dma:
                            ins.then_inc(dsem[(e, op.slot)], 16)
                        elif op.sig:
                            ins.then_inc(csem[e], 1)
                return body

            block.tensor(run("pe"))
            block.scalar(run("act"))
            block.vector(run("dve"))
            block.gpsimd(run("pool"))
            block.sync(run("sp"))

    def dma(self, q, out, in_, R=(), W=(), slow=False):
        if slow:
            return self.add(q, lambda e: e.dma_start(out=out, in_=in_, allow_slow_non_contiguous=True), R, W, dma=True)
        return self.add(q, lambda e: e.dma_start(out=out, in_=in_), R, W, dma=True)

    def mm(self, out, lhsT, rhs, start, stop, R=(), W=()):
        return self.add("pe", lambda e: e.matmul(out, lhsT, rhs, start=start, stop=stop, skip_group_check=True), R, W)

    def tr(self, out, in_, ident, R=(), W=()):
        return self.add("pe", lambda e: e.transpose(out, in_, ident), R, W)

    def act(self, out, in_, func, R=(), W=(), bias=None, scale=None, accum=None):
        kw = {}
        if bias is not None:
            kw["bias"] = bias
        if scale is not None:
            kw["scale"] = scale
        if accum is not None:
            kw["accum_out"] = accum
        return self.add("act", lambda e: e.activation(out, in_, func, **kw), R, W, small=_small(out))

    def ts(self, eng, out, in0, s1, s2, op0, op1=None, R=(), W=(), accum=None, force=False):
        kw = {}
        if op1 is not None:
            kw["op1"] = op1
        if accum is not None:
            kw["accum_out"] = accum
        return self.add(eng, lambda e: e.tensor_scalar(out, in0, s1, s2, op0, **kw), R, W, force=force, small=_small(out))

    def tt(self, eng, out, in0, in1, op, R=(), W=()):
        return self.add(eng, lambda e: e.tensor_tensor(out, in0, in1, op), R, W, small=_small(out))

    def stt(self, eng, out, in0, scalar, in1, op0, op1, R=(), W=()):
        return self.add(eng, lambda e: e.scalar_tensor_tensor(out, in0, scalar, in1, op0, op1), R, W, small=_small(out))

    def copy(self, eng, out, in_, R=(), W=()):
        if eng == "act":
            return self.add("act", lambda e: e.copy(out, in_), R, W, small=_small(out))
        return self.add(eng, lambda e: e.tensor_copy(out, in_), R, W, small=_small(out))

    def memset(self, eng, out, val, R=(), W=()):
        return self.add(eng, lambda e: e.memset(out, val), R, W, small=_small(out))


def _small(ap):
    try:
        return ap.free_size() <= 256
    except Exception:
        return False


class Tile:
    __slots__ = ("ap", "b")

    def __init__(self, ap):
        self.ap = ap
        self.b = Buf()


class Arena:
    def __init__(self, nc, nbytes):
        self.t = nc.alloc_sbuf_tensor("arena", [128, nbytes], U8)
        self.off = 0
        self.cap = nbytes

    def alloc(self, free, dtype, parts=128):
        if isinstance(free, int):
            free = (free,)
        n = 1
        for f in free:
            n *= f
        sz = n * _ISZ[dtype]
        off = (self.off + 63) // 64 * 64
        assert off + sz <= self.cap, ("SBUF arena overflow", off, sz, self.cap)
        self.off = off + sz
        ap = self.t[0:parts, off:off + sz].bitcast(dtype)
        if len(free) > 1:
            names = ["d%d" % i for i in range(len(free))]
            pat = "p (" + " ".join(names) + ") -> p " + " ".join(names)
            ap = ap.rearrange(pat, **{nm: f for nm, f in zip(names, free)})
        return Tile(ap)

    def mark(self):
        return self.off

    def reset(self, m):
        self.off = m


def _bc(ap, n):
    return ap.partition_broadcast(n)


NCST = 704


def make_consts():
    c = np.zeros((128, NCST), np.float32)
    s = np.arange(128)[:, None]
    t = np.arange(128)[None, :]
    same = (s // 64) == (t // 64)
    c[:, 0:128] = np.eye(128, dtype=np.float32)
    c[:, 128:256] = (same & (s <= t))
    c[:, 256:384] = (same & (s > t))
    c[:, 384:512] = (same & (s >= t))
    c[:, 512:640] = (same & (s < t))
    c[:, 640] = (np.arange(128) // 64 == 0)
    c[:, 641] = (np.arange(128) // 64 == 1)
    return c


def make_rope():
    pos = np.arange(T_LAT)
    row = (pos // 64).astype(np.float32)
    col = (pos % 64).astype(np.float32)
    inv = (10000.0 ** (-np.arange(16, dtype=np.float32) / 16)).astype(np.float32)
    ar = row[:, None] * inv[None, :]
    ac = col[:, None] * inv[None, :]
    C = np.concatenate([np.cos(ar), np.cos(ar), np.cos(ac), np.cos(ac)], axis=1)
    Sn = np.concatenate([-np.sin(ar), np.sin(ar), -np.sin(ac), np.sin(ac)], axis=1)
    return np.concatenate([np.tile(C, (1, 8)), np.tile(Sn, (1, 8))], axis=1).astype(np.float32)


class G:
    pass


def hg_half(g, d, hf, zsrc, zbufs, qsb, vsb, OMLd, o_evac):
    S, PS, Tm = g.S, g.PS, g.Tm
    hs = slice(hf * 512, (hf + 1) * 512)
    sg, kk, lf, eb, enb, er, qd, ki, ke, qdT, kiT, ATs, dec = Tm
    S.act(sg.ap, zsrc, AF.Sigmoid, R=zbufs, W=[sg.b], scale=-1.0)
    S.tt("dve", kk.ap, sg.ap, OMLd.ap[:, hs], ALU.mult, R=[sg.b, OMLd.b], W=[kk.b])
    S.act(lf.ap, kk.ap, AF.Ln, R=[kk.b], W=[lf.b], scale=-1.0, bias=g.ONE.ap[:, 0:1])
    P_b, P_r, P_d = PS[4], PS[5], PS[1]
    S.mm(P_b.ap, g.TRI[d], lf.ap, True, True, R=[lf.b, g.CST.b], W=[P_b.b])
    S.mm(P_r.ap, g.TRIX[d], lf.ap, True, True, R=[lf.b], W=[P_r.b])
    for hh in range(4):
        S.mm(P_d.ap[:, hh * 2:hh * 2 + 2], lf.ap[:, hh * 128:(hh + 1) * 128], g.CSEL, True, True, R=[lf.b], W=[P_d.b])
    S.act(dec.ap, P_d.ap[:, 0:8], AF.Exp, R=[P_d.b], W=[dec.b])
    S.act(eb.ap, P_b.ap, AF.Exp, R=[P_b.b], W=[eb.b])
    S.tt("dve", qd.ap, qsb.ap[:, hs], eb.ap, ALU.mult, R=[qsb.b, eb.b], W=[qd.b])
    S.act(enb.ap, P_b.ap, AF.Exp, R=[P_b.b], W=[enb.b], scale=-1.0)
    S.tt("dve", ki.ap, kk.ap, enb.ap, ALU.mult, R=[kk.b, enb.b], W=[ki.b])
    S.act(er.ap, P_r.ap, AF.Exp, R=[P_r.b], W=[er.b])
    S.tt("dve", ke.ap, kk.ap, er.ap, ALU.mult, R=[kk.b, er.b], W=[ke.b])
    for src, dst, P in ((qd, qdT, PS[0]), (ki, kiT, PS[1])):
        Pv = P.ap.bitcast(BF16)
        for hh in range(4):
            S.tr(Pv[:, hh * 128:(hh + 1) * 128], src.ap[:, hh * 128:(hh + 1) * 128], g.IDB.ap, R=[src.b, g.IDB.b], W=[P.b])
        S.copy("act", dst.ap, Pv[:, 0:512], R=[P.b], W=[dst.b])
    P_at, P_o, P_kv = PS[6], PS[7], PS[5]
    for hh in range(4):
        c4 = slice(hh * 128, (hh + 1) * 128)
        S.mm(P_at.ap[:, c4], kiT.ap[:, c4], qdT.ap[:, c4], True, True, R=[kiT.b, qdT.b], W=[P_at.b])
    for hh in range(4):
        c4 = slice(hh * 128, (hh + 1) * 128)
        S.tt("dve", ATs.ap[:, c4], P_at.ap[:, c4], g.TRI[d], ALU.mult, R=[P_at.b], W=[ATs.b])
    P_ox = PS[4]
    for hh in range(4):
        h = hf * 4 + hh
        c4 = slice(hh * 128, (hh + 1) * 128)
        S.mm(P_o.ap[:, c4], ATs.ap[:, c4], vsb.ap[:, h * 128:(h + 1) * 128], True, True, R=[ATs.b, vsb.b], W=[P_o.b])
    order = (0, 1) if d == 0 else (1, 0)
    for ci, c in enumerate(order):
        rows = slice(c * 64, (c + 1) * 64)
        for hh in range(4):
            h = hf * 4 + hh
            S.mm(P_ox.ap[rows, hh * 128:(hh + 1) * 128], qdT.ap[:, hh * 128 + c * 64:hh * 128 + (c + 1) * 64], g.Sbf[h].ap,
                 True, True, R=[qdT.b, g.Sbf[h].b], W=[P_ox.b])
        for hh in range(4):
            h = hf * 4 + hh
            S.mm(P_kv.ap[:, hh * 128:(hh + 1) * 128], ke.ap[rows, hh * 128:(hh + 1) * 128], vsb.ap[rows, h * 128:(h + 1) * 128],
                 True, True, R=[ke.b, vsb.b], W=[P_kv.b])
        for hh in range(4):
            h = hf * 4 + hh
            S.stt("dve", g.Sst[h].ap, g.Sst[h].ap, dec.ap[:, hh * 2 + c:hh * 2 + c + 1], P_kv.ap[:, hh * 128:(hh + 1) * 128],
                  ALU.mult, ALU.add, R=[g.Sst[h].b, dec.b, P_kv.b], W=[g.Sst[h].b])
            S.copy("act", g.Sbf[h].ap, g.Sst[h].ap, R=[g.Sst[h].b], W=[g.Sbf[h].b])
    o_evac(hf, P_o, P_ox)


def alloc_hg_tmp(g):
    A = g.A
    f = [A.alloc(512, F32) for _ in range(6)]
    b = [A.alloc(512, BF16) for _ in range(6)]
    dec = A.alloc(8, F32)
    g.Tm = f + b + [dec]
    g.Sst = [A.alloc(128, F32) for _ in range(8)]
    g.Sbf = [A.alloc(128, BF16) for _ in range(8)]
    for h in range(8):
        g.S.memset("dve", g.Sst[h].ap, 0.0, W=[g.Sst[h].b])
        g.S.memset("dve", g.Sbf[h].ap, 0.0, W=[g.Sbf[h].b])


def load_row(g, dst, row, R=()):
    g.S.dma("sp", dst.ap, row.partition_broadcast(128), R=list(R), W=[dst.b])


def layer_norm_rows(g, r, LNG, LNB, outt):
    S = g.S
    st, mv, rstd = g.ln_st, g.ln_mv, g.ln_rstd
    S.add("dve", lambda e: e.tensor_reduce(st.ap[:, 0:1], r.ap, AX.X, ALU.add), R=[r.b], W=[st.b], small=True)
    S.tt("pool", outt.ap, r.ap, r.ap, ALU.mult, R=[r.b], W=[outt.b])
    S.add("dve", lambda e: e.tensor_reduce(st.ap[:, 1:2], outt.ap, AX.X, ALU.add), R=[outt.b], W=[st.b], small=True)
    S.ts("dve", mv.ap[:, 0:1], st.ap[:, 0:1], 1.0 / D, None, ALU.mult, R=[st.b], W=[mv.b])
    S.stt("dve", st.ap[:, 2:3], mv.ap[:, 0:1], -1.0, mv.ap[:, 0:1], ALU.mult, ALU.mult, R=[mv.b], W=[st.b])
    S.stt("dve", mv.ap[:, 1:2], st.ap[:, 1:2], 1.0 / D, st.ap[:, 2:3], ALU.mult, ALU.add, R=[st.b, mv.b], W=[mv.b])
    S.act(rstd.ap, mv.ap[:, 1:2], AF.Sqrt, R=[mv.b], W=[rstd.b], bias=g.EPSLN.ap[:, 0:1])
    S.add("dve", lambda e: e.reciprocal(rstd.ap, rstd.ap), R=[rstd.b], W=[rstd.b], small=True)
    S.ts("dve", r.ap, r.ap, mv.ap[:, 0:1], rstd.ap[:, 0:1], ALU.subtract, ALU.mult, R=[r.b, mv.b, rstd.b], W=[r.b], force=True)
    S.tt("pool", r.ap, r.ap, LNG.ap, ALU.mult, R=[r.b, LNG.b], W=[r.b])
    S.tt("pool", outt.ap, r.ap, LNB.ap, ALU.add, R=[r.b, LNB.b], W=[outt.b])


def mixer_tail(g, t, ybf, xt, r_idx, rows, WOUT, WR, BR, X1, H2T, GATES, yT_in=None):
    S, PS = g.S, g.PS
    G1, LNG, LNB, SC2, SH2 = rows
    yT, t1, rr, x1, h2, h2Tf, h2Tb, lg, top8, msk, negm, ee, zz = g.tail_tmp
    if yT_in is not None:
        yT = yT_in
    else:
        P = PS[0]
        Pv = P.ap.bitcast(BF16)
        for k in range(8):
            S.tr(Pv[:, k * 128:(k + 1) * 128], ybf.ap[:, k * 128:(k + 1) * 128], g.IDB.ap, R=[ybf.b, g.IDB.b], W=[P.b])
        S.copy("act", yT.ap, Pv, R=[P.b], W=[yT.b])
    for n in range(2):
        Pm = PS[2 + n]
        ns = slice(n * 512, (n + 1) * 512)
        for k in range(8):
            S.mm(Pm.ap, yT.ap[:, k * 128:(k + 1) * 128], WOUT.ap[:, k, ns], k == 0, k == 7, R=[yT.b, WOUT.b], W=[Pm.b])
        S.tt("dve", t1.ap[:, ns], Pm.ap, G1[r_idx].ap[:, ns], ALU.mult, R=[Pm.b, G1[r_idx].b], W=[t1.b])
    S.stt("dve", rr.ap, xt.ap, ALPHA, t1.ap, ALU.mult, ALU.add, R=[xt.b, t1.b], W=[rr.b])
    if g.cut == 6 and t == 65:
        dbg = g.dbg
        S.dma("sp", dbg[0:128, :], G1[r_idx].ap, R=[G1[r_idx].b])
        S.dma("sp", dbg[128:256, :], t1.ap, R=[t1.b])
        S.dma("sp", dbg[256:384, :], rr.ap, R=[rr.b])
        S.dma("sp", dbg[384:512, :], xt.ap, R=[xt.b])
        ms_, osb_, g2_, sq_ = g.dbg_extra
        S.dma("sp", dbg[512:640, 0:8], ms_.ap, R=[ms_.b])
        S.dma("sp", dbg[640:768, :], osb_.ap, R=[osb_.b])
        S.dma("sp", dbg[768:896, :], g2_.ap, R=[g2_.b])
        S.dma("sp", dbg[896:1024, :], sq_.ap, R=[sq_.b])
        g.early = True
        return
    layer_norm_rows(g, rr, LNG, LNB, x1)
    S.dma("sp", X1[t * 128:(t + 1) * 128, :], x1.ap, R=[x1.b], W=[g.X1b[t]])
    if g.cut == 3:
        return
    S.tt("dve", h2.ap, x1.ap, SC2[r_idx].ap, ALU.mult, R=[x1.b, SC2[r_idx].b], W=[h2.b])
    S.tt("dve", h2.ap, h2.ap, SH2[r_idx].ap, ALU.add, R=[h2.b, SH2[r_idx].b], W=[h2.b])
    for gi in range(2):
        Pt = PS[4 + gi]
        for j in range(4):
            k = gi * 4 + j
            S.tr(Pt.ap[:, j * 128:(j + 1) * 128], h2.ap[:, k * 128:(k + 1) * 128], g.ident, R=[h2.b, g.CST.b], W=[Pt.b])
        S.copy("dve", h2Tf.ap[:, gi * 512:(gi + 1) * 512], Pt.ap, R=[Pt.b], W=[h2Tf.b])
    S.copy("act", h2Tb.ap, h2Tf.ap, R=[h2Tf.b], W=[h2Tb.b])
    S.dma("sp", H2T[t].rearrange("p k n -> p (k n)"), h2Tb.ap, R=[h2Tb.b], W=[g.H2Tb[t]])
    if g.cut == 4:
        return
    Pl = PS[6]
    for k in range(8):
        S.mm(Pl.ap[:, 0:32], h2Tf.ap[:, k * 128:(k + 1) * 128], WR.ap[:, k, :], k == 0, k == 7, R=[h2Tf.b, WR.b], W=[Pl.b])
    S.tt("dve", lg.ap, Pl.ap[:, 0:32], BR.ap, ALU.add, R=[Pl.b, BR.b], W=[lg.b])
    S.add("dve", lambda e: e.max(top8.ap, lg.ap), R=[lg.b], W=[top8.b], small=True)
    S.ts("dve", msk.ap, lg.ap, top8.ap[:, 3:4], None, ALU.is_ge, R=[lg.b, top8.b], W=[msk.b], force=True)
    S.ts("dve", negm.ap, top8.ap[:, 0:1], -1.0, None, ALU.mult, R=[top8.b], W=[negm.b])
    S.act(ee.ap, lg.ap, AF.Exp, R=[lg.b, negm.b], W=[ee.b], bias=negm.ap[:, 0:1])
    S.tt("dve", ee.ap, ee.ap, msk.ap, ALU.mult, R=[ee.b, msk.b], W=[ee.b])
    S.add("dve", lambda e: e.tensor_reduce(zz.ap, ee.ap, AX.X, ALU.add), R=[ee.b], W=[zz.b], small=True)
    S.add("dve", lambda e: e.reciprocal(negm.ap, zz.ap), R=[zz.b], W=[negm.b], small=True)
    S.act(msk.ap, ee.ap, AF.Identity, R=[ee.b, negm.b], W=[msk.b], scale=negm.ap[:, 0:1])
    S.dma("sp", GATES[t * 128:(t + 1) * 128, :], msk.ap, R=[msk.b], W=[g.GATESb[t]])


def alloc_tail_tmp(g):
    A = g.A
    yT = A.alloc(1024, BF16)
    t1 = A.alloc(1024, F32)
    rr = A.alloc(1024, F32)
    x1 = A.alloc(1024, F32)
    h2 = A.alloc(1024, F32)
    h2Tf = A.alloc(1024, F32)
    h2Tb = A.alloc(1024, BF16)
    lg = A.alloc(32, F32)
    top8 = A.alloc(8, F32)
    msk = A.alloc(32, F32)
    negm = A.alloc(1, F32)
    ee = A.alloc(32, F32)
    zz = A.alloc(1, F32)
    g.tail_tmp = (yT, t1, rr, x1, h2, h2Tf, h2Tb, lg, top8, msk, negm, ee, zz)
    g.ln_st = A.alloc(12, F32)
    g.ln_mv = A.alloc(2, F32)
    g.ln_rstd = A.alloc(1, F32)


def build(stop=None):
    nc = bass.Bass("TRN2", target_bir_lowering=False)
    g = G()
    g.nc = nc
    import os
    g.cut = int(os.environ.get("KCUT", "0"))

    early = stop in ("modA", "p1", "l0mix", "p1dbg", "p2sim")
    nc.in_names = []

    def din(name, shape, dt=F32):
        if early and name in ("moe_w_gu", "moe_w_dn", "da_w_in", "da_w_out", "rope"):
            return None
        if stop == "l0moe" and name in ("da_w_in", "da_w_out", "rope"):
            return None
        nc.in_names.append(name)
        return nc.dram_tensor(name, list(shape), dt, kind="ExternalInput").ap()

    def dsc(name, shape, dt=F32):
        if stop == "all" and name in ("X1", "X2", "X3", "YT", "GATES"):
            return nc.dram_tensor(name, list(shape), dt, kind="ExternalOutput").ap()
        return nc.dram_tensor(name, list(shape), dt).ap()

    x = din("x", [T_LAT, D])
    ctx = din("ctx", [T_CTX, D])
    c2 = din("c2", [2, D])
    w_ada = din("w_ada", [2, D, 6 * D])
    b_ada = din("b_ada", [2, 6 * D])
    ln_g = din("ln_g", [2, 2, D])
    ln_b = din("ln_b", [2, 2, D])
    hg_w_in = din("hg_w_in", [1, D, 5 * D])
    hg_lb = din("hg_lb", [2, 3, D])
    hg_norm_g = din("hg_norm_g", [1, D])
    hg_w_out = din("hg_w_out", [1, D, D])
    da_w_in = din("da_w_in", [1, D, 3 * D])
    da_lam = din("da_lam", [1, 4, 64])
    da_norm_g = din("da_norm_g", [1, 128])
    da_w_out = din("da_w_out", [1, D, D])
    moe_w_router = din("moe_w_router", [2, D, NE])
    moe_b_router = din("moe_b_router", [2, NE])
    moe_w_gu = din("moe_w_gu", [2, NE, D, 2 * D])
    moe_b_gu = din("moe_b_gu", [2, NE, 2 * D])
    moe_w_dn = din("moe_w_dn", [2, NE, D, D])
    moe_b_dn = din("moe_b_dn", [2, NE, D])
    cst = din("cst", [128, NCST])
    rope = din("rope", [T_LAT, 1024])
    out = nc.dram_tensor("out", [T_LAT, D], F32, kind="ExternalOutput").ap()
    dbg = None
    if stop is not None and stop != "all":
        dbg = nc.dram_tensor("dbg", [T_ALL, D], F32, kind="ExternalOutput").ap()

    g.dbg = dbg
    MOD = dsc("MOD", [2, 2, 6 * D])
    MODb = Buf()
    Qs = dsc("Qs", [T_ALL, D])
    ZBs = dsc("ZBs", [T_ALL, D])
    Gs = dsc("Gs", [T_ALL, D])
    Vs = dsc("Vs", [T_ALL, D], BF16)
    OFs = dsc("OFs", [T_ALL, D])
    p1b = [Buf() for _ in range(NT)]
    X1 = dsc("X1", [T_ALL, D])
    H2T = dsc("H2T", [NT, 128, 8, 128], BF16)
    GATES = dsc("GATES", [T_ALL, NE])
    g.X1b = [Buf() for _ in range(NT)]
    g.H2Tb = [Buf() for _ in range(NT)]
    g.GATESb = [Buf() for _ in range(NT)]

    S = Sched()
    g.S = S
    A = Arena(nc, 200 * 1024)
    g.A = A
    g.PS = [Tile(nc.alloc_psum_tensor("ps%d" % i, [128, 512], F32)[:, :]) for i in range(8)]
    PS = g.PS

    def tok_src(t):
        if t < 2:
            return ctx[t * 128:(t + 1) * 128, :]
        return x[(t - 2) * 128:(t - 1) * 128, :]

    g.CST = A.alloc(NCST, F32)
    S.dma("sp", g.CST.ap, cst, W=[g.CST.b])
    g.IDB = A.alloc(128, BF16)
    S.copy("dve", g.IDB.ap, g.CST.ap[:, 0:128], R=[g.CST.b], W=[g.IDB.b])
    g.ONE = A.alloc(1, F32)
    S.memset("dve", g.ONE.ap, 1.0, W=[g.ONE.b])
    g.EPSLN = A.alloc(1, F32)
    S.memset("dve", g.EPSLN.ap, LN_EPS, W=[g.EPSLN.b])
    g.EPSRMS = A.alloc(1, F32)
    S.memset("dve", g.EPSRMS.ap, RMS_EPS, W=[g.EPSRMS.b])
    g.ident = g.CST.ap[:, 0:128]
    g.TRI = {0: g.CST.ap[:, 128:256], 1: g.CST.ap[:, 384:512]}
    g.TRIX = {0: g.CST.ap[:, 256:384], 1: g.CST.ap[:, 512:640]}
    g.CSEL = g.CST.ap[:, 640:642]

    m0 = A.mark()
    cT = A.alloc(16, F32)
    scT = A.alloc(16, F32)
    cTv = cT.ap.rearrange("p (k r) -> p k r", r=2)
    for r in range(2):
        S.dma("sp", cTv[:, :, r], c2[r].rearrange("(c p) -> p c", p=128), W=[cT.b], slow=True)
    S.act(scT.ap, cT.ap, AF.Silu, R=[cT.b], W=[scT.b])
    wb = [A.alloc(3072, F32) for _ in range(2)]
    bb = A.alloc(3072, F32, parts=2)
    msb = A.alloc(3072, F32, parts=2)
    for l in range(2):
        for hf in range(2):
            cs = slice(hf * 3072, (hf + 1) * 3072)
            S.dma("sp", bb.ap, b_ada[l, cs].partition_broadcast(2), W=[bb.b])
            for k in range(8):
                w = wb[k % 2]
                S.dma("sp", w.ap, w_ada[l, k * 128:(k + 1) * 128, cs], W=[w.b])
                for j in range(6):
                    S.mm(PS[j].ap[0:2, :], scT.ap[:, k * 2:k * 2 + 2], w.ap[:, j * 512:(j + 1) * 512], k == 0, k == 7,
                         R=[scT.b, w.b], W=[PS[j].b])
            for j in range(6):
                S.tt("dve", msb.ap[:, j * 512:(j + 1) * 512], PS[j].ap[0:2, :], bb.ap[:, j * 512:(j + 1) * 512], ALU.add,
                     R=[PS[j].b, bb.b], W=[msb.b])
            S.dma("sp", MOD[l, :, cs], msb.ap, R=[msb.b], W=[MODb])
    A.reset(m0)
    S.barrier()
    if stop == "modA":
        S.dma("sp", dbg[0:4, :].rearrange("a (b d) -> (a b) d", b=1), MOD[0:2, :, 0:1024].rearrange("l r d -> (l r) d"), R=[MODb])
        S.finish()
        S.emit(nc)
        return nc

    def modrow(l, r, i):
        return MOD[l, r, i * 1024:(i + 1) * 1024]

    base_mark = A.mark()
    OML = [A.alloc(D, F32) for _ in range(2)]
    mt = A.mark()
    lbt = A.alloc(6 * D, F32)
    lv = lbt.ap.rearrange("p (a d) -> p a d", a=6)
    mx = A.alloc(D, F32)
    S.dma("sp", lbt.ap, hg_lb.rearrange("a b d -> (a b d)").partition_broadcast(128), W=[lbt.b])
    for d in range(2):
        S.tt("dve", mx.ap, lv[:, 3 * d, :], lv[:, 3 * d + 1, :], ALU.max, R=[lbt.b], W=[mx.b])
        S.tt("dve", mx.ap, mx.ap, lv[:, 3 * d + 2, :], ALU.max, R=[lbt.b, mx.b], W=[mx.b])
        for i in range(3):
            S.tt("dve", lv[:, 3 * d + i, :], lv[:, 3 * d + i, :], mx.ap, ALU.subtract, R=[lbt.b, mx.b], W=[lbt.b])
    S.act(lbt.ap, lbt.ap, AF.Exp, R=[lbt.b], W=[lbt.b])
    for d in range(2):
        S.tt("dve", OML[d].ap, lv[:, 3 * d + 1, :], lv[:, 3 * d + 2, :], ALU.add, R=[lbt.b], W=[OML[d].b])
        S.tt("dve", mx.ap, OML[d].ap, lv[:, 3 * d, :], ALU.add, R=[lbt.b, OML[d].b], W=[mx.b])
        S.add("dve", lambda e: e.reciprocal(mx.ap, mx.ap), R=[mx.b], W=[mx.b], small=True)
        S.tt("dve", OML[d].ap, OML[d].ap, mx.ap, ALU.mult, R=[OML[d].b, mx.b], W=[OML[d].b])
    A.reset(mt)
    S.barrier()

    m1 = A.mark()
    WIN = A.alloc(8 * 5 * D, BF16)
    WINv = WIN.ap.rearrange("p (k n) -> p k n", k=8)
    for k in range(8):
        S.dma("pool", WINv[:, k, :], hg_w_in[0, k * 128:(k + 1) * 128, :], W=[WIN.b])
    SC1 = [A.alloc(D, F32) for _ in range(2)]
    SH1 = [A.alloc(D, F32) for _ in range(2)]
    for r in range(2):
        load_row(g, SH1[r], modrow(0, r, 0), R=[MODb])
        load_row(g, SC1[r], modrow(0, r, 1), R=[MODb])
        S.ts("dve", SC1[r].ap, SC1[r].ap, 1.0, None, ALU.add, R=[SC1[r].b], W=[SC1[r].b])
    xb = [A.alloc(D, F32) for _ in range(2)]
    hb = A.alloc(D, F32)
    hT = A.alloc(D, BF16)
    qsb = A.alloc(D, F32)
    vsb = A.alloc(D, BF16)
    gsb = A.alloc(D, F32)
    zbsb = A.alloc(D, F32)
    ofsb = A.alloc(D, F32)
    alloc_hg_tmp(g)

    def evac1(hf, P_o, P_ox):
        hs = slice(hf * 512, (hf + 1) * 512)
        S.copy("act", ofsb.ap[:, hs], P_o.ap, R=[P_o.b], W=[ofsb.b])
        S.tt("dve", ofsb.ap[:, hs], ofsb.ap[:, hs], P_ox.ap, ALU.add, R=[ofsb.b, P_ox.b], W=[ofsb.b])

    chunk_order = [0, 1, 2, 3, 4, 5, 8, 9, 6, 7]
    for t in range(NT if stop != 'p2sim' else 0):
        r = 1 if t < 2 else 0
        xt = xb[t % 2]
        S.dma("sp", xt.ap, tok_src(t), W=[xt.b])
        S.tt("dve", hb.ap, xt.ap, SC1[r].ap, ALU.mult, R=[xt.b, SC1[r].b], W=[hb.b])
        S.tt("dve", hb.ap, hb.ap, SH1[r].ap, ALU.add, R=[hb.b, SH1[r].b], W=[hb.b])
        for gi in range(2):
            P = PS[gi]
            for j in range(4):
                k = gi * 4 + j
                S.tr(P.ap[:, j * 128:(j + 1) * 128], hb.ap[:, k * 128:(k + 1) * 128], g.ident, R=[hb.b, g.CST.b], W=[P.b])
            S.copy("act", hT.ap[:, gi * 512:(gi + 1) * 512], P.ap, R=[P.b], W=[hT.b])
        zP = {}
        for ci, cidx in enumerate(chunk_order):
            P = PS[2 + ci % 2]
            for k in range(8):
                S.mm(P.ap, hT.ap[:, k * 128:(k + 1) * 128], WINv[:, k, cidx * 512:(cidx + 1) * 512], k == 0, k == 7,
                     R=[hT.b, WIN.b], W=[P.b])
            typ, hf = divmod(cidx, 2)
            hs = slice(hf * 512, (hf + 1) * 512)
            if typ == 0:
                S.copy("act", qsb.ap[:, hs], P.ap, R=[P.b], W=[qsb.b])
            elif typ == 1:
                S.copy("act", vsb.ap[:, hs], P.ap, R=[P.b], W=[vsb.b])
            elif typ == 2:
                S.copy("act", gsb.ap[:, hs], P.ap, R=[P.b], W=[gsb.b])
            elif typ == 4:
                S.copy("act", zbsb.ap[:, hs], P.ap, R=[P.b], W=[zbsb.b])
            else:
                zP[hf] = P
        for hf in range(2):
            hg_half(g, 0, hf, zP[hf].ap, [zP[hf].b], qsb, vsb, OML[0], evac1)
            if stop == "p1dbg" and t == 0 and hf == 0:
                sg, kk, lf, eb, enb, er, qd, ki, ke, qdT, kiT, ATs, dec = g.Tm
                S.dma("sp", dbg[0:128, :], hb.ap, R=[hb.b])
                S.dma("sp", dbg[128:256, :], qsb.ap, R=[qsb.b])
                S.dma("sp", dbg[256:384, :], gsb.ap, R=[gsb.b])
                S.dma("sp", dbg[384:512, :], zbsb.ap, R=[zbsb.b])
                for i, tl in enumerate((sg, kk, lf, eb, enb, er)):
                    S.dma("sp", dbg[512 + 128 * (i // 2):640 + 128 * (i // 2), (i % 2) * 512:(i % 2) * 512 + 512], tl.ap, R=[tl.b])
                S.dma("sp", dbg[896:1024, :], ofsb.ap, R=[ofsb.b])
                S.dma("sp", dbg[1152:1280, :], OML[0].ap, R=[OML[0].b])
        rs = slice(t * 128, (t + 1) * 128)
        S.dma("sp", Qs[rs, :], qsb.ap, R=[qsb.b], W=[p1b[t]])
        S.dma("sp", Vs[rs, :], vsb.ap, R=[vsb.b], W=[p1b[t]])
        S.dma("sp", Gs[rs, :], gsb.ap, R=[gsb.b], W=[p1b[t]])
        S.dma("sp", ZBs[rs, :], zbsb.ap, R=[zbsb.b], W=[p1b[t]])
        S.dma("sp", OFs[rs, :], ofsb.ap, R=[ofsb.b], W=[p1b[t]])
    A.reset(m1)
    S.barrier()
    if stop == "p1dbg":
        S.finish()
        S.emit(nc)
        return nc
    if stop == "p1":
        for t in range(NT):
            S.dma("sp", dbg[t * 128:(t + 1) * 128, :], OFs[t * 128:(t + 1) * 128, :], R=[p1b[t]])
        S.finish()
        S.emit(nc)
        return nc

    m2 = A.mark()
    WOUT = A.alloc(8 * D, BF16)
    WOUTv = Tile(WOUT.ap.rearrange("p (k n) -> p k n", k=8))
    WOUTv.b = WOUT.b
    for k in range(8):
        S.dma("pool", WOUTv.ap[:, k, :], hg_w_out[0, k * 128:(k + 1) * 128, :], W=[WOUT.b])
    WR = A.alloc(8 * NE, F32)
    WRv = Tile(WR.ap.rearrange("p (k n) -> p k n", k=8))
    WRv.b = WR.b
    for k in range(8):
        S.dma("sp", WRv.ap[:, k, :], moe_w_router[0, k * 128:(k + 1) * 128, :], W=[WR.b])
    BR = A.alloc(NE, F32)
    load_row(g, BR, moe_b_router[0])
    G1 = [A.alloc(D, F32) for _ in range(2)]
    SC2 = [A.alloc(D, F32) for _ in range(2)]
    SH2 = [A.alloc(D, F32) for _ in range(2)]
    for r in range(2):
        load_row(g, G1[r], modrow(0, r, 2), R=[MODb])
        load_row(g, SH2[r], modrow(0, r, 3), R=[MODb])
        load_row(g, SC2[r], modrow(0, r, 4), R=[MODb])
        S.ts("dve", SC2[r].ap, SC2[r].ap, 1.0, None, ALU.add, R=[SC2[r].b], W=[SC2[r].b])
    LNG = A.alloc(D, F32)
    LNB = A.alloc(D, F32)
    NG = A.alloc(D, F32)
    load_row(g, LNG, ln_g[0, 0])
    load_row(g, LNB, ln_b[0, 0])
    load_row(g, NG, hg_norm_g[0])
    ld = [[A.alloc(D, F32) for _ in range(5)] + [A.alloc(D, BF16)] for _ in range(2)]
    osb = A.alloc(D, F32)
    sq = A.alloc(D, F32)
    ms = A.alloc(8, F32)
    ybf = A.alloc(D, BF16)
    alloc_hg_tmp(g)
    alloc_tail_tmp(g)

    order2 = [1, 0] + list(range(NT - 1, 1, -1))
    if stop == 'p2sim':
        order2 = [1, 65]
    for i, t in enumerate(order2):
        r = 1 if t < 2 else 0
        q2, zb2, g2, of2, x2, v2 = ld[i % 2]
        rs = slice(t * 128, (t + 1) * 128)
        S.dma("sp", q2.ap, Qs[rs, :], R=[p1b[t]], W=[q2.b])
        S.dma("sp", zb2.ap, ZBs[rs, :], R=[p1b[t]], W=[zb2.b])
        S.dma("sp", g2.ap, Gs[rs, :], R=[p1b[t]], W=[g2.b])
        S.dma("sp", of2.ap, OFs[rs, :], R=[p1b[t]], W=[of2.b])
        S.dma("sp", v2.ap, Vs[rs, :], R=[p1b[t]], W=[v2.b])
        S.dma("sp", x2.ap, tok_src(t), W=[x2.b])

        def evac2(hf, P_o, P_ox, of2=of2):
            hs = slice(hf * 512, (hf + 1) * 512)
            S.tt("dve", osb.ap[:, hs], P_o.ap, of2.ap[:, hs], ALU.add, R=[P_o.b, of2.b], W=[osb.b])
            S.tt("dve", osb.ap[:, hs], osb.ap[:, hs], P_ox.ap, ALU.add, R=[osb.b, P_ox.b], W=[osb.b])

        for hf in range(2):
            hs = slice(hf * 512, (hf + 1) * 512)
            hg_half(g, 1, hf, zb2.ap[:, hs], [zb2.b], q2, v2, OML[1], evac2)
            if g.cut == 5 and i == 0 and hf == 0:
                sg_, kk_, lf_, eb_, enb_, er_ = g.Tm[0:6]
                S.dma("sp", dbg[0:128, :], q2.ap, R=[q2.b])
                S.dma("sp", dbg[128:256, :], zb2.ap, R=[zb2.b])
                S.dma("sp", dbg[256:384, :], of2.ap, R=[of2.b])
                for ii, tl in enumerate((sg_, kk_, lf_, eb_, enb_, er_)):
                    S.dma("sp", dbg[512 + 128 * (ii // 2):640 + 128 * (ii // 2), (ii % 2) * 512:(ii % 2) * 512 + 512], tl.ap, R=[tl.b])
                S.dma("sp", dbg[896:1024, :], osb.ap, R=[osb.b])
                S.dma("sp", dbg[1152:1280, :], OML[1].ap, R=[OML[1].b])
                S.finish()
                S.emit(nc)
                return nc
        if g.cut == 1:
            S.dma("sp", dbg[rs, :], osb.ap, R=[osb.b])
            continue
        S.tt("pool", sq.ap, osb.ap, osb.ap, ALU.mult, R=[osb.b], W=[sq.b])
        S.add("dve", lambda e: e.tensor_reduce(ms.ap, sq.ap.rearrange("p (h d) -> p h d", h=8), AX.X, ALU.add), R=[sq.b], W=[ms.b], small=True)
        S.act(ms.ap, ms.ap, AF.Sqrt, R=[ms.b], W=[ms.b], bias=g.EPSRMS.ap[:, 0:1], scale=1.0 / 128.0)
        S.add("dve", lambda e: e.reciprocal(ms.ap, ms.ap), R=[ms.b], W=[ms.b], small=True)
        S.act(g2.ap, g2.ap, AF.Silu, R=[g2.b], W=[g2.b])
        for h in range(8):
            hc = slice(h * 128, (h + 1) * 128)
            S.ts("dve", sq.ap[:, hc], osb.ap[:, hc], ms.ap[:, h:h + 1], None, ALU.mult, R=[osb.b, ms.b], W=[sq.b], force=(h == 0))
        S.tt("pool", g2.ap, g2.ap, NG.ap, ALU.mult, R=[g2.b, NG.b], W=[g2.b])
        S.tt("dve", ybf.ap, sq.ap, g2.ap, ALU.mult, R=[sq.b, g2.b], W=[ybf.b])
        if g.cut == 2:
            S.dma("sp", dbg[rs, :], osb.ap, R=[osb.b])
            continue
        g.dbg_extra = (ms, osb, g2, sq)
        mixer_tail(g, t, ybf, x2, r, (G1, LNG, LNB, SC2, SH2), WOUTv, WRv, BR, X1, H2T, GATES)
        if getattr(g, "early", False):
            S.finish()
            S.emit(nc)
            return nc
    if g.cut in (1, 2):
        S.finish()
        S.emit(nc)
        return nc
    A.reset(m2)
    S.barrier()

    if stop in ("l0mix", "p2sim"):
        for t in (range(NT) if stop == "l0mix" else order2):
            S.dma("sp", dbg[t * 128:(t + 1) * 128, :], X1[t * 128:(t + 1) * 128, :], R=[g.X1b[t]])
        S.finish()
        S.emit(nc)
        return nc

    A.reset(base_mark)
    X2 = dsc("X2", [T_ALL, D])
    X2b = [Buf() for _ in range(NT)]
    mdr = (moe_w_gu, moe_b_gu, moe_w_dn, moe_b_dn, ln_g, ln_b, H2T, GATES)
    moe_tiles = list(range(NT))
    if g.cut == 11:
        moe_tiles = list(range(11))
    moe_phase(g, 0, moe_tiles, mdr, X1, g.X1b, lambda t: X2[t * 128:(t + 1) * 128, :], lambda t: X2b[t], MODb, modrow)
    if stop == "l0moe":
        for t in moe_tiles:
            S.dma("sp", dbg[t * 128:(t + 1) * 128, :], X2[t * 128:(t + 1) * 128, :], R=[X2b[t]])
            if len(moe_tiles) <= 11:
                S.dma("sp", dbg[(20 + t) * 128:(21 + t) * 128, 0:32], GATES[t * 128:(t + 1) * 128, :], R=[g.GATESb[t]])
        S.finish()
        S.emit(nc)
        return nc

    A.reset(base_mark)
    QT = dsc("QT", [16, 128, T_LAT], BF16)
    KT = dsc("KT", [16, 128, T_ALL], BF16)
    VB = dsc("VB", [T_ALL, D], BF16)
    YT = dsc("YT", [8, 128, T_LAT], BF16)
    qkvb = Buf()
    YTb = Buf()
    attn_prep(g, (da_w_in, rope), X2, X2b, QT, KT, VB, qkvb, MODb, modrow)
    attn_phase(g, (da_lam, da_norm_g), QT, KT, VB, YT, qkvb, YTb)
    if stop == "l1attn":
        for h in range(8):
            S.dma("sp", dbg[h * 128:(h + 1) * 128, :], YT[h, :, 0:1024], R=[YTb])
        S.finish()
        S.emit(nc)
        return nc

    m3 = A.mark()
    X3 = dsc("X3", [T_ALL, D])
    X3b = [Buf() for _ in range(NT)]
    g.X1b = X3b
    WOUT = A.alloc(8 * D, BF16)
    WOUTv = Tile(WOUT.ap.rearrange("p (k n) -> p k n", k=8))
    WOUTv.b = WOUT.b
    for k in range(8):
        S.dma("pool", WOUTv.ap[:, k, :], da_w_out[0, k * 128:(k + 1) * 128, :], W=[WOUT.b])
    WR = A.alloc(8 * NE, F32)
    WRv = Tile(WR.ap.rearrange("p (k n) -> p k n", k=8))
    WRv.b = WR.b
    for k in range(8):
        S.dma("sp", WRv.ap[:, k, :], moe_w_router[1, k * 128:(k + 1) * 128, :], W=[WR.b])
    BR = A.alloc(NE, F32)
    load_row(g, BR, moe_b_router[1])
    G1 = [A.alloc(D, F32)]
    SC2 = [A.alloc(D, F32)]
    SH2 = [A.alloc(D, F32)]
    load_row(g, G1[0], modrow(1, 0, 2), R=[MODb])
    load_row(g, SH2[0], modrow(1, 0, 3), R=[MODb])
    load_row(g, SC2[0], modrow(1, 0, 4), R=[MODb])
    S.ts("dve", SC2[0].ap, SC2[0].ap, 1.0, None, ALU.add, R=[SC2[0].b], W=[SC2[0].b])
    LNG = A.alloc(D, F32)
    LNB = A.alloc(D, F32)
    load_row(g, LNG, ln_g[1, 0])
    load_row(g, LNB, ln_b[1, 0])
    yTl = [A.alloc(D, BF16) for _ in range(2)]
    x2l = [A.alloc(D, F32) for _ in range(2)]
    alloc_tail_tmp(g)
    for t in range(2, NT):
        yt_ = yTl[t % 2]
        xt_ = x2l[t % 2]
        tok0 = (t - 2) * 128
        S.dma("sp", yt_.ap.rearrange("p (k n) -> p k n", k=8), YT[:, :, tok0:tok0 + 128].rearrange("k p n -> p k n"), R=[YTb], W=[yt_.b])
        S.dma("sp", xt_.ap, X2[t * 128:(t + 1) * 128, :], R=[X2b[t]], W=[xt_.b])
        mixer_tail(g, t, None, xt_, 0, (G1, LNG, LNB, SC2, SH2), WOUTv, WRv, BR, X3, H2T, GATES, yT_in=yt_)
    A.reset(m3)
    S.barrier()
    if stop == "l1mix":
        for t in range(2, NT):
            S.dma("sp", dbg[t * 128:(t + 1) * 128, :], X3[t * 128:(t + 1) * 128, :], R=[X3b[t]])
        S.finish()
        S.emit(nc)
        return nc

    A.reset(base_mark)
    outb = [Buf() for _ in range(NT)]
    moe_phase(g, 1, list(range(2, NT)), mdr, X3, X3b, lambda t: out[(t - 2) * 128:(t - 1) * 128, :], lambda t: outb[t], MODb, modrow)
    S.finish()
    S.emit(nc)
    return nc


def make_in_maps(inputs, names=None):
    cst = make_consts()
    rope = make_rope()
    shared = {k: np.ascontiguousarray(inputs[k]) for k in (
        "w_ada", "b_ada", "ln_g", "ln_b", "hg_w_in", "hg_lb", "hg_norm_g", "hg_w_out", "da_w_in", "da_lam",
        "da_norm_g", "da_w_out", "moe_w_router", "moe_b_router", "moe_w_gu", "moe_b_gu", "moe_w_dn", "moe_b_dn")}
    shared["cst"] = cst
    shared["rope"] = rope
    maps = []
    for b in range(N_CORES):
        m = dict(shared)
        m["x"] = np.ascontiguousarray(inputs["x"][b])
        m["ctx"] = np.ascontiguousarray(inputs["ctx"][b])
        m["c2"] = np.ascontiguousarray(np.stack([inputs["c"][b], inputs["c_ctx"]], axis=0))
        if names is not None:
            m = {k: v for k, v in m.items() if k in names}
        maps.append(m)
    return maps


def kernel(**inputs):
    nc = build()
    maps = make_in_maps(inputs)
    res = run_bass_kernel_spmd(nc, maps, core_ids=list(range(N_CORES)))
    return np.stack([np.asarray(r["out"], dtype=np.float32) for r in res.results], axis=0)


def moe_phase(g, l, tiles, dr, X1, X1b, dst_of, dst_b_of, MODb, modrow):
    S, A, PS = g.S, g.A, g.PS
    mk = A.mark()
    w_gu, b_gu, w_dn, b_dn, ln_g, ln_b, H2T, GATES = dr
    GMAX = 11
    BG32 = A.alloc(2048, F32, parts=32)
    S.dma("sp", BG32.ap, b_gu[l], W=[BG32.b])
    BGUT = A.alloc(512, F32)
    Pt = PS[0]
    for fc in range(16):
        S.tr(Pt.ap[:, fc * 32:(fc + 1) * 32], BG32.ap[:, fc * 128:(fc + 1) * 128], g.CST.ap[0:32, 0:32], R=[BG32.b, g.CST.b], W=[Pt.b])
    S.copy("dve", BGUT.ap, Pt.ap, R=[Pt.b], W=[BGUT.b])
    BDN = A.alloc(1024, F32, parts=32)
    S.dma("sp", BDN.ap, b_dn[l], W=[BDN.b])
    G2 = [A.alloc(D, F32) for _ in range(2)]
    for r in range(2):
        load_row(g, G2[r], modrow(l, r, 5), R=[MODb])
    LNG = A.alloc(D, F32)
    LNB = A.alloc(D, F32)
    load_row(g, LNG, ln_g[l, 1])
    load_row(g, LNB, ln_b[l, 1])
    h2T = A.alloc(GMAX * 1024, BF16)
    h2Tv = h2T.ap.rearrange("p (t k n) -> p t k n", t=GMAX, k=8)
    gt = A.alloc(GMAX * NE, F32)
    GT = A.alloc(GMAX * 128, F32, parts=32)
    acc = A.alloc(GMAX * 1024, F32)
    accb = [Buf() for _ in range(GMAX)]
    WG = [A.alloc(8 * 2 * 512, BF16) for _ in range(2)]
    WD = [A.alloc(4 * 1024, BF16) for _ in range(2)]
    gc = [A.alloc(512, F32) for _ in range(2)]
    sg = [A.alloc(512, F32) for _ in range(2)]
    u0 = [A.alloc(512, F32) for _ in range(2)]
    tg = [A.alloc(512, F32) for _ in range(2)]
    actT = [A.alloc(4 * 512, BF16) for _ in range(2)]
    ysc = [A.alloc(512, F32) for _ in range(2)]
    xl = [A.alloc(D, F32) for _ in range(2)]
    rr = A.alloc(D, F32)
    xo = A.alloc(D, F32)
    g.ln_st = A.alloc(12, F32)
    g.ln_mv = A.alloc(2, F32)
    g.ln_rstd = A.alloc(1, F32)

    groups = [tiles[i:i + GMAX] for i in range(0, len(tiles), GMAX)]
    wcount = [0]

    def load_w(e, hx, b):
        wg = WG[b].ap.rearrange("p (k c f) -> p k c f", k=8, c=2)
        for c in range(2):
            S.dma("pool", wg[:, :, c, :],
                  w_gu[l, e, :, c * 1024 + hx * 512:c * 1024 + hx * 512 + 512].rearrange("(k p) f -> p k f", p=128), W=[WG[b].b])
        wd = WD[b].ap.rearrange("p (j n) -> p j n", j=4)
        S.dma("pool", wd, w_dn[l, e, hx * 512:(hx + 1) * 512, :].rearrange("(j p) n -> p j n", p=128), W=[WD[b].b])

    for grp in groups:
        G_ = len(grp)
        for ti, t in enumerate(grp):
            S.dma("sp", h2T.ap[:, ti * 1024:(ti + 1) * 1024], H2T[t].rearrange("p k n -> p (k n)"), R=[g.H2Tb[t]], W=[h2T.b])
            S.dma("sp", gt.ap[:, ti * NE:(ti + 1) * NE], GATES[t * 128:(t + 1) * 128, :], R=[g.GATESb[t]], W=[gt.b])
        seq = [(e, hx) for e in range(NE) for hx in range(2)]
        load_w(seq[0][0], seq[0][1], 0)
        for ti in range(G_):
            Pq = PS[1]
            S.tr(Pq.ap[0:32, 0:128], gt.ap[:, ti * NE:(ti + 1) * NE], g.ident, R=[gt.b, g.CST.b], W=[Pq.b])
            S.copy("dve", GT.ap[:, ti * 128:(ti + 1) * 128], Pq.ap[0:32, 0:128], R=[Pq.b], W=[GT.b])
            for n in range(2):
                Py = PS[2 + n]
                S.mm(Py.ap, GT.ap[:, ti * 128:(ti + 1) * 128], BDN.ap[:, n * 512:(n + 1) * 512], True, True, R=[GT.b, BDN.b], W=[Py.b])
                S.copy("act", acc.ap[:, ti * 1024 + n * 512:ti * 1024 + (n + 1) * 512], Py.ap, R=[Py.b], W=[accb[ti]])
        chunks = [(c0, min(4, G_ - c0)) for c0 in range(0, G_, 4)]
        for si, (e, hx) in enumerate(seq):
            b = si % 2
            if si + 1 < len(seq):
                load_w(seq[si + 1][0], seq[si + 1][1], 1 - b)
            wg = WG[b].ap.rearrange("p (k c f) -> p k c f", k=8, c=2)
            wd = WD[b].ap.rearrange("p (j n) -> p j n", j=4)
            for ci, (c0, nt) in enumerate(chunks):
                N = nt * 128
                aT = actT[ci % 2]
                aTv = aT.ap.rearrange("p (j n) -> p j n", j=4)
                for j in range(4):
                    pb = j % 2
                    Pg, Pu = PS[pb * 2], PS[pb * 2 + 1]
                    for c, P in ((0, Pg), (1, Pu)):
                        for k in range(8):
                            S.mm(P.ap[:, 0:N], wg[:, k, c, j * 128:(j + 1) * 128], h2Tv[:, c0:c0 + nt, k, :], k == 0, k == 7,
                                 R=[WG[b].b, h2T.b], W=[P.b])
                    fc = hx * 4 + j
                    bg = BGUT.ap[:, fc * 32 + e:fc * 32 + e + 1]
                    bu = BGUT.ap[:, (8 + fc) * 32 + e:(8 + fc) * 32 + e + 1]
                    S.ts("dve", gc[pb].ap[:, 0:N], Pg.ap[:, 0:N], bg, 7.0, ALU.add, ALU.min, R=[Pg.b, BGUT.b], W=[gc[pb].b])
                    S.act(sg[pb].ap[:, 0:N], gc[pb].ap[:, 0:N], AF.Sigmoid, R=[gc[pb].b], W=[sg[pb].b], scale=1.702)
                    S.act(u0[pb].ap[:, 0:N], Pu.ap[:, 0:N], AF.Identity, R=[Pu.b, BGUT.b], W=[u0[pb].b], bias=bu)
                    S.ts("dve", u0[pb].ap[:, 0:N], u0[pb].ap[:, 0:N], 7.0, -7.0, ALU.min, ALU.max, R=[u0[pb].b], W=[u0[pb].b])
                    S.tt("pool", tg[pb].ap[:, 0:N], gc[pb].ap[:, 0:N], sg[pb].ap[:, 0:N], ALU.mult, R=[gc[pb].b, sg[pb].b], W=[tg[pb].b])
                    S.stt("dve", aTv[:, j, 0:N], u0[pb].ap[:, 0:N], 1.0, tg[pb].ap[:, 0:N], ALU.add, ALU.mult,
                          R=[u0[pb].b, tg[pb].b], W=[aT.b])
                for tl in range(nt):
                    ti = c0 + tl
                    for n in range(2):
                        Py = PS[4 + (tl * 2 + n) % 4]
                        for j in range(4):
                            S.mm(Py.ap, aTv[:, j, tl * 128:(tl + 1) * 128], wd[:, j, n * 512:(n + 1) * 512], j == 0, j == 3,
                                 R=[aT.b, WD[b].b], W=[Py.b])
                        asl = acc.ap[:, ti * 1024 + n * 512:ti * 1024 + (n + 1) * 512]
                        yb = ysc[(tl * 2 + n) % 2]
                        S.act(yb.ap, Py.ap, AF.Identity, R=[Py.b, gt.b], W=[yb.b], scale=gt.ap[:, ti * NE + e:ti * NE + e + 1])
                        S.tt("dve", asl, asl, yb.ap, ALU.add, R=[yb.b, accb[ti]], W=[accb[ti]])
        for ti, t in enumerate(grp):
            r = 1 if t < 2 else 0
            xt = xl[ti % 2]
            S.dma("sp", xt.ap, X1[t * 128:(t + 1) * 128, :], R=[X1b[t]], W=[xt.b])
            S.tt("dve", xo.ap, acc.ap[:, ti * 1024:(ti + 1) * 1024], G2[r].ap, ALU.mult, R=[accb[ti], G2[r].b], W=[xo.b])
            S.stt("dve", rr.ap, xt.ap, ALPHA, xo.ap, ALU.mult, ALU.add, R=[xt.b, xo.b], W=[rr.b])
            layer_norm_rows(g, rr, LNG, LNB, xo)
            S.dma("sp", dst_of(t), xo.ap, R=[xo.b], W=[dst_b_of(t)])
    A.reset(mk)
    S.barrier()


def attn_prep(g, dr, X2, X2b, QT, KT, VB, qkvb, MODb, modrow):
    S, A, PS = g.S, g.A, g.PS
    da_w_in, rope = dr
    mk = A.mark()
    DAW = A.alloc(8 * 3 * D, BF16)
    DAWv = DAW.ap.rearrange("p (k n) -> p k n", k=8)
    for k in range(8):
        S.dma("pool", DAWv[:, k, :], da_w_in[0, k * 128:(k + 1) * 128, :], W=[DAW.b])
    SC1 = [A.alloc(D, F32) for _ in range(2)]
    SH1 = [A.alloc(D, F32) for _ in range(2)]
    for r in range(2):
        load_row(g, SH1[r], modrow(1, r, 0), R=[MODb])
        load_row(g, SC1[r], modrow(1, r, 1), R=[MODb])
        S.ts("dve", SC1[r].ap, SC1[r].ap, 1.0, None, ALU.add, R=[SC1[r].b], W=[SC1[r].b])
    xb = [A.alloc(D, F32) for _ in range(2)]
    rp = [A.alloc(D, F32) for _ in range(2)]
    hb = A.alloc(D, F32)
    hT = A.alloc(D, BF16)
    qr = A.alloc(512, F32)
    t2 = A.alloc(512, F32)
    sq = A.alloc(512, F32)
    n2 = A.alloc(8, F32)
    qa = A.alloc(8 * 128, BF16)
    ka = A.alloc(8 * 128, BF16)
    qav = qa.ap.rearrange("p (h d) -> p h d", h=8)
    kav = ka.ap.rearrange("p (h d) -> p h d", h=8)
    S.memset("dve", qa.ap, 0.0, W=[qa.b])
    S.memset("dve", ka.ap, 0.0, W=[ka.b])
    aT = [A.alloc(8 * 128, BF16) for _ in range(2)]
    vsb = A.alloc(D, BF16)
    kmx = A.alloc(16, F32)
    S.memset("dve", kmx.ap, 0.0, W=[kmx.b])
    g.kmx = kmx

    def rope_chunk(P, rpt, ch, do_rope):
        if not do_rope:
            S.copy("act", qr.ap, P.ap, R=[P.b], W=[qr.b])
            return
        C8 = rpt.ap[:, 0:512]
        S8 = rpt.ap[:, 512:1024]
        S.tt("dve", qr.ap, P.ap, C8, ALU.mult, R=[P.b, rpt.b], W=[qr.b])
        Pv = P.ap.rearrange("p (a b d) -> p a b d", b=2, d=16)
        t2v = t2.ap.rearrange("p (a b d) -> p a b d", b=2, d=16)
        S8v = S8.rearrange("p (a b d) -> p a b d", b=2, d=16)
        for ab in range(2):
            S.tt("dve", t2v[:, :, ab, :], Pv[:, :, 1 - ab, :], S8v[:, :, ab, :], ALU.mult, R=[P.b, rpt.b], W=[t2.b])
        S.tt("pool", qr.ap, qr.ap, t2.ap, ALU.add, R=[qr.b, t2.b], W=[qr.b])

    for t in range(NT):
        r = 1 if t < 2 else 0
        lat = t >= 2
        xt = xb[t % 2]
        rpt = rp[t % 2]
        S.dma("sp", xt.ap, X2[t * 128:(t + 1) * 128, :], R=[X2b[t]], W=[xt.b])
        if lat:
            S.dma("sp", rpt.ap, rope[(t - 2) * 128:(t - 1) * 128, :], W=[rpt.b])
        S.tt("dve", hb.ap, xt.ap, SC1[r].ap, ALU.mult, R=[xt.b, SC1[r].b], W=[hb.b])
        S.tt("dve", hb.ap, hb.ap, SH1[r].ap, ALU.add, R=[hb.b, SH1[r].b], W=[hb.b])
        for gi in range(2):
            P = PS[gi]
            for j in range(4):
                k = gi * 4 + j
                S.tr(P.ap[:, j * 128:(j + 1) * 128], hb.ap[:, k * 128:(k + 1) * 128], g.ident, R=[hb.b, g.CST.b], W=[P.b])
            S.copy("act", hT.ap[:, gi * 512:(gi + 1) * 512], P.ap, R=[P.b], W=[hT.b])
        chunks = ([0, 1] if lat else []) + [2, 3, 4, 5]
        for ci, cidx in enumerate(chunks):
            P = PS[2 + ci % 2]
            for k in range(8):
                S.mm(P.ap, hT.ap[:, k * 128:(k + 1) * 128], DAWv[:, k, cidx * 512:(cidx + 1) * 512], k == 0, k == 7,
                     R=[hT.b, DAW.b], W=[P.b])
            typ, hf = divmod(cidx, 2)
            if typ == 2:
                S.copy("act", vsb.ap[:, hf * 512:(hf + 1) * 512], P.ap, R=[P.b], W=[vsb.b])
                continue
            rope_chunk(P, rpt, hf, lat)
            S.tt("pool", sq.ap, qr.ap, qr.ap, ALU.mult, R=[qr.b], W=[sq.b])
            S.add("dve", lambda e: e.tensor_reduce(n2.ap, sq.ap.rearrange("p (h d) -> p h d", h=8), AX.X, ALU.add), R=[sq.b], W=[n2.b], small=True)
            qrv = qr.ap.rearrange("p (h d) -> p h d", h=8)
            if typ == 0:
                av, at_ = qav, qa
                S.act(av[:, :, 0:64], qrv, AF.Identity, R=[qr.b], W=[at_.b], scale=0.125)
                S.act(n2.ap, n2.ap, AF.Sqrt, R=[n2.b], W=[n2.b], scale=1.0 / 64.0)
                S.ts("dve", av[:, :, 64], n2.ap, -1.0, None, ALU.mult, R=[n2.b], W=[at_.b])
                dstT = QT
                tok0 = (t - 2) * 128
            else:
                av, at_ = kav, ka
                S.copy("act", av[:, :, 0:64], qrv, R=[qr.b], W=[at_.b])
                S.tt("dve", kmx.ap[:, hf * 8:(hf + 1) * 8], kmx.ap[:, hf * 8:(hf + 1) * 8], n2.ap, ALU.max, R=[kmx.b, n2.b], W=[kmx.b])
                dstT = KT
                tok0 = t * 128
            Pt = PS[4 + ci % 2]
            Ptv = Pt.ap.bitcast(BF16)
            for hm in range(8):
                S.tr(Ptv[:, hm * 128:(hm + 1) * 128], av[:, hm, :], g.IDB.ap, R=[at_.b, g.IDB.b], W=[Pt.b])
            a_t = aT[ci % 2]
            S.copy("act", a_t.ap, Ptv, R=[Pt.b], W=[a_t.b])
            S.dma("sp", dstT[hf * 8:(hf + 1) * 8, :, tok0:tok0 + 128].rearrange("h p n -> p h n"),
                  a_t.ap.rearrange("p (h n) -> p h n", h=8), R=[a_t.b], W=[qkvb])
        S.dma("sp", VB[t * 128:(t + 1) * 128, :], vsb.ap, R=[vsb.b], W=[qkvb])
    A.reset(mk)
    S.barrier()


def attn_phase(g, dr, QT, KT, VB, YT, qkvb, YTb):
    S, A, PS = g.S, g.A, g.PS
    da_lam, da_norm_g = dr
    mk = A.mark()
    kmx = g.kmx
    Pk = PS[0]
    S.tr(Pk.ap[0:16, 0:128], kmx.ap, g.ident, R=[kmx.b, g.CST.b], W=[Pk.b])
    kv = A.alloc(1, F32, parts=16)
    S.add("dve", lambda e: e.tensor_reduce(kv.ap, Pk.ap[0:16, 0:128], AX.X, ALU.max), R=[Pk.b], W=[kv.b], small=True)
    S.act(kv.ap, kv.ap, AF.Sqrt, R=[kv.b], W=[kv.b])
    dg = A.alloc(16, F32, parts=16)
    S.ts("dve", dg.ap, g.CST.ap[0:16, 0:16], kv.ap[:, 0:1], None, ALU.mult, R=[g.CST.b, kv.b], W=[dg.b])
    on16 = A.alloc(128, F32, parts=16)
    S.memset("dve", on16.ap, 1.0, W=[on16.b])
    Pk2 = PS[1]
    S.mm(Pk2.ap[:, 0:16], on16.ap, dg.ap, True, True, R=[on16.b, dg.b], W=[Pk2.b])
    KMB = A.alloc(16, F32)
    S.copy("dve", KMB.ap, Pk2.ap[:, 0:16], R=[Pk2.b], W=[KMB.b])
    lp = A.alloc(256, F32)
    S.dma("sp", lp.ap, da_lam[0].rearrange("a d -> (a d)").partition_broadcast(128), W=[lp.b])
    pr = A.alloc(128, F32)
    lpv = lp.ap.rearrange("p (a b d) -> p a b d", a=2, b=2)
    prv = pr.ap.rearrange("p (a d) -> p a d", a=2)
    S.tt("dve", prv, lpv[:, :, 0, :], lpv[:, :, 1, :], ALU.mult, R=[lp.b], W=[pr.b])
    ls = A.alloc(2, F32)
    S.add("dve", lambda e: e.tensor_reduce(ls.ap, prv, AX.X, ALU.add), R=[pr.b], W=[ls.b], small=True)
    S.act(ls.ap, ls.ap, AF.Exp, R=[ls.b], W=[ls.b])
    NLAM = A.alloc(1, F32)
    S.tt("dve", NLAM.ap, ls.ap[:, 1:2], ls.ap[:, 0:1], ALU.subtract, R=[ls.b], W=[NLAM.b])
    S.ts("dve", NLAM.ap, NLAM.ap, -LAM_INIT1, None, ALU.add, R=[NLAM.b], W=[NLAM.b])
    NG1 = A.alloc(1, F32)
    S.dma("sp", NG1.ap, da_norm_g[0].rearrange("(p o) -> p o", o=1), W=[NG1.b])
    S.ts("dve", NG1.ap, NG1.ap, 1.0 - LAM_INIT1, None, ALU.mult, R=[NG1.b], W=[NG1.b])
    onc = A.alloc(1, F32)
    S.memset("dve", onc.ap, 1.0, W=[onc.b])
    onr = A.alloc(128, F32, parts=1)
    S.memset("dve", onr.ap, 1.0, W=[onr.b])

    Vh = A.alloc(NT * 128, BF16)
    kt = [A.alloc(T_ALL, BF16) for _ in range(2)]
    qt = [A.alloc(512, BF16) for _ in range(2)]
    pT = [A.alloc(512, BF16) for _ in range(3)]
    ZP = [A.alloc(512, F32) for _ in range(2)]
    ZD = [A.alloc(512, F32) for _ in range(2)]
    Bc = [A.alloc(512, F32) for _ in range(2)]
    rz = [A.alloc(512, F32, parts=1) for _ in range(2)]
    oT = A.alloc(512, F32)
    tmp = A.alloc(512, F32)
    yT = [A.alloc(512, BF16) for _ in range(2)]
    pcount = 0
    for h in range(8):
        S.dma("sp", Vh.ap.rearrange("p (t d) -> p t d", d=128), VB[:, h * 128:(h + 1) * 128].rearrange("(t p) d -> p t d", p=128),
              R=[qkvb], W=[Vh.b])
        for c in range(2):
            hm = 2 * h + c
            S.dma("sp", kt[c].ap, KT[hm], R=[qkvb], W=[kt[c].b])
            S.act(kt[c].ap[64:65, :], kt[c].ap[64:65, :], AF.Identity, R=[kt[c].b, KMB.b], W=[kt[c].b], bias=KMB.ap[64:65, hm:hm + 1])
        for qg in range(T_LAT // 512):
            for c in range(2):
                hm = 2 * h + c
                S.dma("sp", qt[c].ap, QT[hm, :, qg * 512:(qg + 1) * 512], R=[qkvb], W=[qt[c].b])
                Po = PS[2 + c]
                for ki in range(NT):
                    Psc = PS[ki % 2]
                    S.mm(Psc.ap, kt[c].ap[:, ki * 128:(ki + 1) * 128], qt[c].ap, True, True, R=[kt[c].b, qt[c].b], W=[Psc.b])
                    p = pT[pcount % 3]
                    pcount += 1
                    S.act(p.ap, Psc.ap, AF.Exp, R=[Psc.b], W=[p.b])
                    S.mm(Po.ap, Vh.ap[:, ki * 128:(ki + 1) * 128], p.ap, ki == 0, ki == NT - 1, R=[Vh.b, p.b], W=[Po.b])
                    if ki % 2 == 0:
                        eng, Z = "pool", ZP[c]
                    else:
                        eng, Z = "dve", ZD[c]
                    if ki < 2:
                        S.copy(eng, Z.ap, p.ap, R=[p.b], W=[Z.b])
                    else:
                        S.tt(eng, Z.ap, Z.ap, p.ap, ALU.add, R=[Z.b, p.b], W=[Z.b])
                Pz = PS[4 + c]
                S.mm(Pz.ap[0:1, :], onc.ap, ZP[c].ap, True, False, R=[onc.b, ZP[c].b], W=[Pz.b])
                S.mm(Pz.ap[0:1, :], onc.ap, ZD[c].ap, False, True, R=[onc.b, ZD[c].b], W=[Pz.b])
                S.add("dve", lambda e, c=c, Pz=Pz: e.reciprocal(rz[c].ap, Pz.ap[0:1, :]), R=[Pz.b], W=[rz[c].b], small=True)
                if c == 1:
                    S.ts("dve", rz[c].ap, rz[c].ap, NLAM.ap[0:1, 0:1], None, ALU.mult, R=[rz[c].b, NLAM.b], W=[rz[c].b])
                Pb = PS[6 + c]
                S.mm(Pb.ap, onr.ap, rz[c].ap, True, True, R=[onr.b, rz[c].b], W=[Pb.b])
                S.copy("act", Bc[c].ap, Pb.ap, R=[Pb.b], W=[Bc[c].b])
            S.tt("dve", oT.ap, PS[2].ap, Bc[0].ap, ALU.mult, R=[PS[2].b, Bc[0].b], W=[oT.b])
            S.tt("dve", tmp.ap, PS[3].ap, Bc[1].ap, ALU.mult, R=[PS[3].b, Bc[1].b], W=[tmp.b])
            S.tt("pool", oT.ap, oT.ap, tmp.ap, ALU.add, R=[oT.b, tmp.b], W=[oT.b])
            S.tt("pool", tmp.ap, oT.ap, oT.ap, ALU.mult, R=[oT.b], W=[tmp.b])
            Pm = PS[4]
            S.mm(Pm.ap[0:1, :], onc.ap, tmp.ap, True, True, R=[onc.b, tmp.b], W=[Pm.b])
            S.act(rz[0].ap, Pm.ap[0:1, :], AF.Sqrt, R=[Pm.b, g.EPSRMS.b], W=[rz[0].b], bias=g.EPSRMS.ap[0:1, 0:1], scale=1.0 / 128.0)
            S.add("dve", lambda e: e.reciprocal(rz[0].ap, rz[0].ap), R=[rz[0].b], W=[rz[0].b], small=True)
            Pb = PS[6]
            S.mm(Pb.ap, onr.ap, rz[0].ap, True, True, R=[onr.b, rz[0].b], W=[Pb.b])
            y = yT[qg % 2]
            S.stt("dve", y.ap, oT.ap, NG1.ap[:, 0:1], Pb.ap, ALU.mult, ALU.mult, R=[oT.b, NG1.b, Pb.b], W=[y.b])
            S.dma("sp", YT[h, :, qg * 512:(qg + 1) * 512], y.ap, R=[y.b], W=[YTb])
    A.reset(mk)
    S.barrier()
```

```python
import numpy as np
from contextlib import ExitStack
import concourse.bass as bass
import concourse.mybir as mybir
from concourse.bass_utils import run_bass_kernel_spmd

F32 = mybir.dt.float32
BF16 = mybir.dt.bfloat16
U8 = mybir.dt.uint8
AF = mybir.ActivationFunctionType
ALU = mybir.AluOpType
AX = mybir.AxisListType
_ISZ = {F32: 4, BF16: 2}

D = 1024
T_CTX = 256
T_LAT = 8192
T_ALL = T_CTX + T_LAT
NT = T_ALL // 128
NTL = T_LAT // 128
NE = 32
ALPHA = 4.0 ** 0.25
LN_EPS = 1e-5
RMS_EPS = 1e-6
LAM_INIT1 = 0.8 - 0.6 * float(np.exp(-0.3 * 1))
N_CORES = 8
import os as _os
_KDEBUG = bool(_os.environ.get("KDEBUG"))


class Buf:
    __slots__ = ("w", "rd", "rdd", "wx")

    def __init__(self):
        self.w = None
        self.wx = []
        self.rd = {}
        self.rdd = []


class Op:
    __slots__ = ("eng", "fn", "waits", "sig", "idx", "dma", "slot", "semval", "sigval", "tb", "force", "small")


class Sched:
    ENGS = ("pe", "act", "dve", "pool", "sp")
    K = 8

    def __init__(self):
        self.ops = {e: [] for e in self.ENGS}
        self.waited = {e: {} for e in self.ENGS}
        self.dq = {e: [] for e in self.ENGS}
        self.pending = {e: [] for e in self.ENGS}
        self.lastc = {e: None for e in self.ENGS}

    def _dep(self, c, p):
        if p is None:
            return
        w = self.waited[c.eng]
        if p.dma:
            key = ("d", p.eng, p.slot)
            if w.get(key, 0) >= p.semval:
                return
            w[key] = p.semval
            c.waits.append(p)
        else:
            if p.eng == c.eng and not c.dma and not c.force and not p.small:
                return
            key = ("c", p.eng)
            if w.get(key, -1) >= p.idx:
                return
            w[key] = p.idx
            p.sig = True
            c.waits.append(p)

    def add(self, eng, fn, R=(), W=(), dma=False, force=False, small=False):
        op = Op()
        op.force = force
        op.small = small
        op.eng = eng
        op.fn = fn
        op.waits = []
        op.sig = False
        op.dma = dma
        op.slot = 0
        op.semval = 0
        op.sigval = 0
        op.idx = len(self.ops[eng])
        op.tb = None
        if _KDEBUG:
            import traceback
            op.tb = traceback.extract_stack(limit=5)
        if self.pending[eng]:
            for p in self.pending[eng]:
                self._dep(op, p)
            self.pending[eng] = []
        for b in R:
            self._dep(op, b.w)
            for p in b.wx:
                self._dep(op, p)
        for b in W:
            self._dep(op, b.w)
            for p in b.wx:
                self._dep(op, p)
            for p in b.rd.values():
                self._dep(op, p)
            for p in b.rdd:
                self._dep(op, p)
        if dma:
            q = self.dq[eng]
            n = len(q)
            op.slot = n % self.K
            op.semval = 16 * (n // self.K + 1)
            if n >= self.K:
                self._dep(op, q[n - self.K])
            q.append(op)
        for b in R:
            if dma:
                b.rdd.append(op)
            else:
                b.rd[eng] = op
        for b in W:
            if dma and b.w is not None and b.w.dma and not b.rd and not b.rdd:
                b.wx.append(b.w)
            else:
                b.wx = []
            b.w = op
            b.rd = {}
            b.rdd = []
        self.ops[eng].append(op)
        if not dma:
            self.lastc[eng] = op
        return op

    def barrier(self):
        lst = [self.lastc[e] for e in self.ENGS if self.lastc[e] is not None]
        for e in self.ENGS:
            lst += self.dq[e][-self.K:]
        for e in self.ENGS:
            self.pending[e] = list(lst)

    def finish(self):
        self.barrier()
        for e in self.ENGS:
            self.add(e, None)

    def emit(self, nc):
        for e in self.ENGS:
            cnt = 0
            for op in self.ops[e]:
                if op.sig:
                    cnt += 1
                    op.sigval = cnt
        with ExitStack() as st:
            csem = {e: st.enter_context(nc.semaphore("c_" + e)) for e in self.ENGS}
            dsem = {}
            for e in self.ENGS:
                if self.dq[e]:
                    for k in range(self.K):
                        dsem[(e, k)] = st.enter_context(nc.semaphore("d_%s%d" % (e, k)))
            block = st.enter_context(nc.Block())

            def run(e):
                def body(eng):
                    for op in self.ops[e]:
                        for p in op.waits:
                            if p.dma:
                                eng.wait_ge(dsem[(p.eng, p.slot)], p.semval)
                            else:
                                eng.wait_ge(csem[p.eng], p.sigval)
                        if op.fn is None:
                            continue
                        try:
                            ins = op.fn(eng)
                        except Exception:
                            if op.tb is not None:
                                print("FAILED OP from:", [(f.lineno, f.line) for f in op.tb[:-1]])
                            raise
                        if op.dma:
                            ins.then_inc(dsem[(e, op.slot)], 16)
                        elif op.sig:
                            ins.then_inc(csem[e], 1)
                return body

            block.tensor(run("pe"))
            block.scalar(run("act"))
            block.vector(run("dve"))
            block.gpsimd(run("pool"))
            block.sync(run("sp"))

    def dma(self, q, out, in_, R=(), W=(), slow=False):
        if slow:
            return self.add(q, lambda e: e.dma_start(out=out, in_=in_, allow_slow_non_contiguous=True), R, W, dma=True)
        return self.add(q, lambda e: e.dma_start(out=out, in_=in_), R, W, dma=True)

    def mm(self, out, lhsT, rhs, start, stop, R=(), W=()):
        return self.add("pe", lambda e: e.matmul(out, lhsT, rhs, start=start, stop=stop, skip_group_check=True), R, W)

    def tr(self, out, in_, ident, R=(), W=()):
        return self.add("pe", lambda e: e.transpose(out, in_, ident), R, W)

    def act(self, out, in_, func, R=(), W=(), bias=None, scale=None, accum=None):
        kw = {}
        if bias is not None:
            kw["bias"] = bias
        if scale is not None:
            kw["scale"] = scale
        if accum is not None:
            kw["accum_out"] = accum
        return self.add("act", lambda e: e.activation(out, in_, func, **kw), R, W, small=_small(out))

    def ts(self, eng, out, in0, s1, s2, op0, op1=None, R=(), W=(), accum=None, force=False):
        kw = {}
        if op1 is not None:
            kw["op1"] = op1
        if accum is not None:
            kw["accum_out"] = accum
        return self.add(eng, lambda e: e.tensor_scalar(out, in0, s1, s2, op0, **kw), R, W, force=force, small=_small(out))

    def tt(self, eng, out, in0, in1, op, R=(), W=()):
        return self.add(eng, lambda e: e.tensor_tensor(out, in0, in1, op), R, W, small=_small(out))

    def stt(self, eng, out, in0, scalar, in1, op0, op1, R=(), W=()):
        return self.add(eng, lambda e: e.scalar_tensor_tensor(out, in0, scalar, in1, op0, op1), R, W, small=_small(out))

    def copy(self, eng, out, in_, R=(), W=()):
        if eng == "act":
            return self.add("act", lambda e: e.copy(out, in_), R, W, small=_small(out))
        return self.add(eng, lambda e: e.tensor_copy(out, in_), R, W, small=_small(out))

    def memset(self, eng, out, val, R=(), W=()):
        return self.add(eng, lambda e: e.memset(out, val), R, W, small=_small(out))


def _small(ap):
    try:
        return ap.free_size() <= 256
    except Exception:
        return False


class Tile:
    __slots__ = ("ap", "b")

    def __init__(self, ap):
        self.ap = ap
        self.b = Buf()


class Arena:
    def __init__(self, nc, nbytes):
        self.nc = nc
        probe = nc.alloc_sbuf_tensor("arena_probe", [128, 64], U8)
        self.base = int(nc.lookup_mloc(probe).addr) + 64
        self.off = 0
        self.cap = nbytes
        self.n = 0

    def alloc(self, free, dtype, parts=128):
        if isinstance(free, int):
            free = (free,)
        n = 1
        for f in free:
            n *= f
        sz = n * _ISZ[dtype]
        off = (self.off + 63) // 64 * 64
        assert off + sz <= self.cap, ("SBUF arena overflow", off, sz, self.cap)
        self.off = off + sz
        self.n += 1
        h = self.nc.alloc_sbuf_tensor_at("t%d" % self.n, [parts, n], dtype, offset=self.base + off)
        ap = h[:, :]
        if len(free) > 1:
            names = ["d%d" % i for i in range(len(free))]
            pat = "p (" + " ".join(names) + ") -> p " + " ".join(names)
            ap = ap.rearrange(pat, **{nm: f for nm, f in zip(names, free)})
        return Tile(ap)

    def mark(self):
        return self.off

    def reset(self, m):
        self.off = m


def _bc(ap, n):
    return ap.partition_broadcast(n)


NCST = 704


def make_consts():
    c = np.zeros((128, NCST), np.float32)
    s = np.arange(128)[:, None]
    t = np.arange(128)[None, :]
    same = (s // 64) == (t // 64)
    c[:, 0:128] = np.eye(128, dtype=np.float32)
    c[:, 128:256] = (same & (s <= t))
    c[:, 256:384] = (same & (s > t))
    c[:, 384:512] = (same & (s >= t))
    c[:, 512:640] = (same & (s < t))
    c[:, 640] = (np.arange(128) // 64 == 0)
    c[:, 641] = (np.arange(128) // 64 == 1)
    return c


def make_rope():
    pos = np.arange(T_LAT)
    row = (pos // 64).astype(np.float32)
    col = (pos % 64).astype(np.float32)
    inv = (10000.0 ** (-np.arange(16, dtype=np.float32) / 16)).astype(np.float32)
    ar = row[:, None] * inv[None, :]
    ac = col[:, None] * inv[None, :]
    C = np.concatenate([np.cos(ar), np.cos(ar), np.cos(ac), np.cos(ac)], axis=1)
    Sn = np.concatenate([-np.sin(ar), np.sin(ar), -np.sin(ac), np.sin(ac)], axis=1)
    return np.concatenate([np.tile(C, (1, 8)), np.tile(Sn, (1, 8))], axis=1).astype(np.float32)


class G:
    pass


def hg_half(g, d, hf, zsrc, zbufs, qsb, vsb, OMLd, o_evac):
    S, PS, Tm = g.S, g.PS, g.Tm
    hs = slice(hf * 512, (hf + 1) * 512)
    sg, kk, lf, eb, enb, er, qd, ki, ke, qdT, kiT, ATs, dec = Tm
    S.act(sg.ap, zsrc, AF.Sigmoid, R=zbufs, W=[sg.b], scale=-1.0)
    S.tt("dve", kk.ap, sg.ap, OMLd.ap[:, hs], ALU.mult, R=[sg.b, OMLd.b], W=[kk.b])
    S.act(lf.ap, kk.ap, AF.Ln, R=[kk.b], W=[lf.b], scale=-1.0, bias=g.ONE.ap[:, 0:1])
    P_b, P_r, P_d = PS[4], PS[5], PS[1]
    S.mm(P_b.ap, g.TRI[d], lf.ap, True, True, R=[lf.b, g.CST.b], W=[P_b.b])
    S.mm(P_r.ap, g.TRIX[d], lf.ap, True, True, R=[lf.b], W=[P_r.b])
    for hh in range(4):
        S.mm(P_d.ap[:, hh * 2:hh * 2 + 2], lf.ap[:, hh * 128:(hh + 1) * 128], g.CSEL, True, True, R=[lf.b], W=[P_d.b])
    S.act(dec.ap, P_d.ap[:, 0:8], AF.Exp, R=[P_d.b], W=[dec.b])
    S.act(eb.ap, P_b.ap, AF.Exp, R=[P_b.b], W=[eb.b])
    S.tt("dve", qd.ap, qsb.ap[:, hs], eb.ap, ALU.mult, R=[qsb.b, eb.b], W=[qd.b])
    S.act(enb.ap, P_b.ap, AF.Exp, R=[P_b.b], W=[enb.b], scale=-1.0)
    S.tt("dve", ki.ap, kk.ap, enb.ap, ALU.mult, R=[kk.b, enb.b], W=[ki.b])
    S.act(er.ap, P_r.ap, AF.Exp, R=[P_r.b], W=[er.b])
    S.tt("dve", ke.ap, kk.ap, er.ap, ALU.mult, R=[kk.b, er.b], W=[ke.b])
    for src, dst, P in ((qd, qdT, PS[0]), (ki, kiT, PS[1])):
        Pv = P.ap.bitcast(BF16)
        for hh in range(4):
            S.tr(Pv[:, hh * 128:(hh + 1) * 128], src.ap[:, hh * 128:(hh + 1) * 128], g.IDB.ap, R=[src.b, g.IDB.b], W=[P.b])
        S.copy("act", dst.ap, Pv[:, 0:512], R=[P.b], W=[dst.b])
    P_at, P_o, P_kv = PS[6], PS[7], PS[5]
    for hh in range(4):
        c4 = slice(hh * 128, (hh + 1) * 128)
        S.mm(P_at.ap[:, c4], kiT.ap[:, c4], qdT.ap[:, c4], True, True, R=[kiT.b, qdT.b], W=[P_at.b])
    for hh in range(4):
        c4 = slice(hh * 128, (hh + 1) * 128)
        S.tt("dve", ATs.ap[:, c4], P_at.ap[:, c4], g.TRI[d], ALU.mult, R=[P_at.b], W=[ATs.b])
    P_ox = PS[4]
    for hh in range(4):
        h = hf * 4 + hh
        c4 = slice(hh * 128, (hh + 1) * 128)
        S.mm(P_o.ap[:, c4], ATs.ap[:, c4], vsb.ap[:, h * 128:(h + 1) * 128], True, True, R=[ATs.b, vsb.b], W=[P_o.b])
    order = (0, 1) if d == 0 else (1, 0)
    for ci, c in enumerate(order):
        rows = slice(c * 64, (c + 1) * 64)
        for hh in range(4):
            h = hf * 4 + hh
            S.mm(P_ox.ap[rows, hh * 128:(hh + 1) * 128], qdT.ap[:, hh * 128 + c * 64:hh * 128 + (c + 1) * 64], g.Sbf[h].ap,
                 True, True, R=[qdT.b, g.Sbf[h].b], W=[P_ox.b])
        for hh in range(4):
            h = hf * 4 + hh
            S.mm(P_kv.ap[:, hh * 128:(hh + 1) * 128], ke.ap[rows, hh * 128:(hh + 1) * 128], vsb.ap[rows, h * 128:(h + 1) * 128],
                 True, True, R=[ke.b, vsb.b], W=[P_kv.b])
        for hh in range(4):
            h = hf * 4 + hh
            S.stt("dve", g.Sst[h].ap, g.Sst[h].ap, dec.ap[:, hh * 2 + c:hh * 2 + c + 1], P_kv.ap[:, hh * 128:(hh + 1) * 128],
                  ALU.mult, ALU.add, R=[g.Sst[h].b, dec.b, P_kv.b], W=[g.Sst[h].b])
            S.copy("act", g.Sbf[h].ap, g.Sst[h].ap, R=[g.Sst[h].b], W=[g.Sbf[h].b])
    o_evac(hf, P_o, P_ox)


def alloc_hg_tmp(g):
    A = g.A
    f = [A.alloc(512, F32) for _ in range(6)]
    b = [A.alloc(512, BF16) for _ in range(6)]
    dec = A.alloc(8, F32)
    g.Tm = f + b + [dec]
    g.Sst = [A.alloc(128, F32) for _ in range(8)]
    g.Sbf = [A.alloc(128, BF16) for _ in range(8)]
    for h in range(8):
        g.S.memset("dve", g.Sst[h].ap, 0.0, W=[g.Sst[h].b])
        g.S.memset("dve", g.Sbf[h].ap, 0.0, W=[g.Sbf[h].b])


def load_row(g, dst, row, R=()):
    g.S.dma("sp", dst.ap, row.partition_broadcast(128), R=list(R), W=[dst.b])


def layer_norm_rows(g, r, LNG, LNB, outt):
    S = g.S
    st, mv, rstd = g.ln_st, g.ln_mv, g.ln_rstd
    S.add("dve", lambda e: e.tensor_reduce(st.ap[:, 0:1], r.ap, AX.X, ALU.add), R=[r.b], W=[st.b], small=True)
    S.tt("pool", outt.ap, r.ap, r.ap, ALU.mult, R=[r.b], W=[outt.b])
    S.add("dve", lambda e: e.tensor_reduce(st.ap[:, 1:2], outt.ap, AX.X, ALU.add), R=[outt.b], W=[st.b], small=True)
    S.ts("dve", mv.ap[:, 0:1], st.ap[:, 0:1], 1.0 / D, None, ALU.mult, R=[st.b], W=[mv.b])
    S.stt("dve", st.ap[:, 2:3], mv.ap[:, 0:1], -1.0, mv.ap[:, 0:1], ALU.mult, ALU.mult, R=[mv.b], W=[st.b])
    S.stt("dve", mv.ap[:, 1:2], st.ap[:, 1:2], 1.0 / D, st.ap[:, 2:3], ALU.mult, ALU.add, R=[st.b, mv.b], W=[mv.b])
    S.act(rstd.ap, mv.ap[:, 1:2], AF.Sqrt, R=[mv.b], W=[rstd.b], bias=g.EPSLN.ap[:, 0:1])
    S.add("dve", lambda e: e.reciprocal(rstd.ap, rstd.ap), R=[rstd.b], W=[rstd.b], small=True)
    S.ts("dve", r.ap, r.ap, mv.ap[:, 0:1], rstd.ap[:, 0:1], ALU.subtract, ALU.mult, R=[r.b, mv.b, rstd.b], W=[r.b], force=True)
    S.tt("pool", r.ap, r.ap, LNG.ap, ALU.mult, R=[r.b, LNG.b], W=[r.b])
    S.tt("pool", outt.ap, r.ap, LNB.ap, ALU.add, R=[r.b, LNB.b], W=[outt.b])


def mixer_tail(g, t, ybf, xt, r_idx, rows, WOUT, WR, BR, X1, H2T, GATES, yT_in=None):
    S, PS = g.S, g.PS
    G1, LNG, LNB, SC2, SH2 = rows
    yT, t1, rr, x1, h2, h2Tf, h2Tb, lg, top8, msk, negm, ee, zz = g.tail_tmp
    if yT_in is not None:
        yT = yT_in
    else:
        P = PS[0]
        Pv = P.ap.bitcast(BF16)
        for k in range(8):
            S.tr(Pv[:, k * 128:(k + 1) * 128], ybf.ap[:, k * 128:(k + 1) * 128], g.IDB.ap, R=[ybf.b, g.IDB.b], W=[P.b])
        S.copy("act", yT.ap, Pv, R=[P.b], W=[yT.b])
    for n in range(2):
        Pm = PS[2 + n]
        ns = slice(n * 512, (n + 1) * 512)
        for k in range(8):
            S.mm(Pm.ap, yT.ap[:, k * 128:(k + 1) * 128], WOUT.ap[:, k, ns], k == 0, k == 7, R=[yT.b, WOUT.b], W=[Pm.b])
        S.tt("dve", t1.ap[:, ns], Pm.ap, G1[r_idx].ap[:, ns], ALU.mult, R=[Pm.b, G1[r_idx].b], W=[t1.b])
    S.stt("dve", rr.ap, xt.ap, ALPHA, t1.ap, ALU.mult, ALU.add, R=[xt.b, t1.b], W=[rr.b])
    if g.cut == 6 and t == 65:
        dbg = g.dbg
        S.dma("sp", dbg[0:128, :], G1[r_idx].ap, R=[G1[r_idx].b])
        S.dma("sp", dbg[128:256, :], t1.ap, R=[t1.b])
        S.dma("sp", dbg[256:384, :], rr.ap, R=[rr.b])
        S.dma("sp", dbg[384:512, :], xt.ap, R=[xt.b])
        ms_, osb_, g2_, sq_ = g.dbg_extra
        S.dma("sp", dbg[512:640, 0:8], ms_.ap, R=[ms_.b])
        S.dma("sp", dbg[640:768, :], osb_.ap, R=[osb_.b])
        S.dma("sp", dbg[768:896, :], g2_.ap, R=[g2_.b])
        S.dma("sp", dbg[896:1024, :], sq_.ap, R=[sq_.b])
        g.early = True
        return
    layer_norm_rows(g, rr, LNG, LNB, x1)
    S.dma("sp", X1[t * 128:(t + 1) * 128, :], x1.ap, R=[x1.b], W=[g.X1b[t]])
    if g.cut == 3:
        return
    S.tt("dve", h2.ap, x1.ap, SC2[r_idx].ap, ALU.mult, R=[x1.b, SC2[r_idx].b], W=[h2.b])
    S.tt("dve", h2.ap, h2.ap, SH2[r_idx].ap, ALU.add, R=[h2.b, SH2[r_idx].b], W=[h2.b])
    for gi in range(2):
        Pt = PS[4 + gi]
        for j in range(4):
            k = gi * 4 + j
            S.tr(Pt.ap[:, j * 128:(j + 1) * 128], h2.ap[:, k * 128:(k + 1) * 128], g.ident, R=[h2.b, g.CST.b], W=[Pt.b])
        S.copy("dve", h2Tf.ap[:, gi * 512:(gi + 1) * 512], Pt.ap, R=[Pt.b], W=[h2Tf.b])
    S.copy("act", h2Tb.ap, h2Tf.ap, R=[h2Tf.b], W=[h2Tb.b])
    S.dma("sp", H2T[t].rearrange("p k n -> p (k n)"), h2Tb.ap, R=[h2Tb.b], W=[g.H2Tb[t]])
    if g.cut == 4:
        return
    Pl = PS[6]
    for k in range(8):
        S.mm(Pl.ap[:, 0:32], h2Tf.ap[:, k * 128:(k + 1) * 128], WR.ap[:, k, :], k == 0, k == 7, R=[h2Tf.b, WR.b], W=[Pl.b])
    S.tt("dve", lg.ap, Pl.ap[:, 0:32], BR.ap, ALU.add, R=[Pl.b, BR.b], W=[lg.b])
    S.add("dve", lambda e: e.max(top8.ap, lg.ap), R=[lg.b], W=[top8.b], small=True)
    S.ts("dve", msk.ap, lg.ap, top8.ap[:, 3:4], None, ALU.is_ge, R=[lg.b, top8.b], W=[msk.b], force=True)
    S.ts("dve", negm.ap, top8.ap[:, 0:1], -1.0, None, ALU.mult, R=[top8.b], W=[negm.b])
    S.act(ee.ap, lg.ap, AF.Exp, R=[lg.b, negm.b], W=[ee.b], bias=negm.ap[:, 0:1])
    S.tt("dve", ee.ap, ee.ap, msk.ap, ALU.mult, R=[ee.b, msk.b], W=[ee.b])
    S.add("dve", lambda e: e.tensor_reduce(zz.ap, ee.ap, AX.X, ALU.add), R=[ee.b], W=[zz.b], small=True)
    S.add("dve", lambda e: e.reciprocal(negm.ap, zz.ap), R=[zz.b], W=[negm.b], small=True)
    S.act(msk.ap, ee.ap, AF.Identity, R=[ee.b, negm.b], W=[msk.b], scale=negm.ap[:, 0:1])
    S.dma("sp", GATES[t * 128:(t + 1) * 128, :], msk.ap, R=[msk.b], W=[g.GATESb[t]])


def alloc_tail_tmp(g):
    A = g.A
    yT = A.alloc(1024, BF16)
    t1 = A.alloc(1024, F32)
    rr = A.alloc(1024, F32)
    x1 = A.alloc(1024, F32)
    h2 = A.alloc(1024, F32)
    h2Tf = A.alloc(1024, F32)
    h2Tb = A.alloc(1024, BF16)
    lg = A.alloc(32, F32)
    top8 = A.alloc(8, F32)
    msk = A.alloc(32, F32)
    negm = A.alloc(1, F32)
    ee = A.alloc(32, F32)
    zz = A.alloc(1, F32)
    g.tail_tmp = (yT, t1, rr, x1, h2, h2Tf, h2Tb, lg, top8, msk, negm, ee, zz)
    g.ln_st = A.alloc(12, F32)
    g.ln_mv = A.alloc(2, F32)
    g.ln_rstd = A.alloc(1, F32)


def build(stop=None):
    nc = bass.Bass("TRN2", target_bir_lowering=False)
    g = G()
    g.nc = nc
    import os
    g.cut = int(os.environ.get("KCUT", "0"))

    early = stop in ("modA", "p1", "l0mix", "p1dbg", "p2sim")
    nc.in_names = []

    def din(name, shape, dt=F32):
        if early and name in ("moe_w_gu", "moe_w_dn", "da_w_in", "da_w_out", "rope"):
            return None
        if stop == "l0moe" and name in ("da_w_in", "da_w_out", "rope"):
            return None
        nc.in_names.append(name)
        return nc.dram_tensor(name, list(shape), dt, kind="ExternalInput").ap()

    def dsc(name, shape, dt=F32):
        if stop == "all" and name in ("X1", "X2", "X3", "YT", "GATES"):
            return nc.dram_tensor(name, list(shape), dt, kind="ExternalOutput").ap()
        return nc.dram_tensor(name, list(shape), dt).ap()

    x = din("x", [T_LAT, D])
    ctx = din("ctx", [T_CTX, D])
    c2 = din("c2", [2, D])
    w_ada = din("w_ada", [2, D, 6 * D])
    b_ada = din("b_ada", [2, 6 * D])
    ln_g = din("ln_g", [2, 2, D])
    ln_b = din("ln_b", [2, 2, D])
    hg_w_in = din("hg_w_in", [1, D, 5 * D])
    hg_lb = din("hg_lb", [2, 3, D])
    hg_norm_g = din("hg_norm_g", [1, D])
    hg_w_out = din("hg_w_out", [1, D, D])
    da_w_in = din("da_w_in", [1, D, 3 * D])
    da_lam = din("da_lam", [1, 4, 64])
    da_norm_g = din("da_norm_g", [1, 128])
    da_w_out = din("da_w_out", [1, D, D])
    moe_w_router = din("moe_w_router", [2, D, NE])
    moe_b_router = din("moe_b_router", [2, NE])
    moe_w_gu = din("moe_w_gu", [2, NE, D, 2 * D])
    moe_b_gu = din("moe_b_gu", [2, NE, 2 * D])
    moe_w_dn = din("moe_w_dn", [2, NE, D, D])
    moe_b_dn = din("moe_b_dn", [2, NE, D])
    cst = din("cst", [128, NCST])
    rope = din("rope", [T_LAT, 1024])
    out = nc.dram_tensor("out", [T_LAT, D], F32, kind="ExternalOutput").ap()
    dbg = None
    if stop is not None and stop != "all":
        dbg = nc.dram_tensor("dbg", [T_ALL, D], F32, kind="ExternalOutput").ap()

    g.dbg = dbg
    MOD = dsc("MOD", [2, 2, 6 * D])
    MODb = Buf()
    Qs = dsc("Qs", [T_ALL, D])
    ZBs = dsc("ZBs", [T_ALL, D])
    Gs = dsc("Gs", [T_ALL, D])
    Vs = dsc("Vs", [T_ALL, D], BF16)
    OFs = dsc("OFs", [T_ALL, D])
    p1b = [Buf() for _ in range(NT)]
    X1 = dsc("X1", [T_ALL, D])
    H2T = dsc("H2T", [NT, 128, 8, 128], BF16)
    GATES = dsc("GATES", [T_ALL, NE])
    g.X1b = [Buf() for _ in range(NT)]
    g.H2Tb = [Buf() for _ in range(NT)]
    g.GATESb = [Buf() for _ in range(NT)]

    S = Sched()
    g.S = S
    A = Arena(nc, 200 * 1024)
    g.A = A
    g.PS = [Tile(nc.alloc_psum_tensor("ps%d" % i, [128, 512], F32)[:, :]) for i in range(8)]
    PS = g.PS

    def tok_src(t):
        if t < 2:
            return ctx[t * 128:(t + 1) * 128, :]
        return x[(t - 2) * 128:(t - 1) * 128, :]

    g.CST = A.alloc(NCST, F32)
    S.dma("sp", g.CST.ap, cst, W=[g.CST.b])
    g.IDB = A.alloc(128, BF16)
    S.copy("dve", g.IDB.ap, g.CST.ap[:, 0:128], R=[g.CST.b], W=[g.IDB.b])
    g.ONE = A.alloc(1, F32)
    S.memset("dve", g.ONE.ap, 1.0, W=[g.ONE.b])
    g.EPSLN = A.alloc(1, F32)
    S.memset("dve", g.EPSLN.ap, LN_EPS, W=[g.EPSLN.b])
    g.EPSRMS = A.alloc(1, F32)
    S.memset("dve", g.EPSRMS.ap, RMS_EPS, W=[g.EPSRMS.b])
    g.ident = g.CST.ap[:, 0:128]
    g.TRI = {0: g.CST.ap[:, 128:256], 1: g.CST.ap[:, 384:512]}
    g.TRIX = {0: g.CST.ap[:, 256:384], 1: g.CST.ap[:, 512:640]}
    g.CSEL = g.CST.ap[:, 640:642]

    m0 = A.mark()
    cT = A.alloc(16, F32)
    scT = A.alloc(16, F32)
    cTv = cT.ap.rearrange("p (k r) -> p k r", r=2)
    for r in range(2):
        S.dma("sp", cTv[:, :, r], c2[r].rearrange("(c p) -> p c", p=128), W=[cT.b], slow=True)
    S.act(scT.ap, cT.ap, AF.Silu, R=[cT.b], W=[scT.b])
    wb = [A.alloc(3072, F32) for _ in range(2)]
    bb = A.alloc(3072, F32, parts=2)
    msb = A.alloc(3072, F32, parts=2)
    for l in range(2):
        for hf in range(2):
            cs = slice(hf * 3072, (hf + 1) * 3072)
            S.dma("sp", bb.ap, b_ada[l, cs].partition_broadcast(2), W=[bb.b])
            for k in range(8):
                w = wb[k % 2]
                S.dma("sp", w.ap, w_ada[l, k * 128:(k + 1) * 128, cs], W=[w.b])
                for j in range(6):
                    S.mm(PS[j].ap[0:2, :], scT.ap[:, k * 2:k * 2 + 2], w.ap[:, j * 512:(j + 1) * 512], k == 0, k == 7,
                         R=[scT.b, w.b], W=[PS[j].b])
            for j in range(6):
                S.tt("dve", msb.ap[:, j * 512:(j + 1) * 512], PS[j].ap[0:2, :], bb.ap[:, j * 512:(j + 1) * 512], ALU.add,
                     R=[PS[j].b, bb.b], W=[msb.b])
            S.dma("sp", MOD[l, :, cs], msb.ap, R=[msb.b], W=[MODb])
    A.reset(m0)
    S.barrier()
    if stop == "modA":
        S.dma("sp", dbg[0:4, :].rearrange("a (b d) -> (a b) d", b=1), MOD[0:2, :, 0:1024].rearrange("l r d -> (l r) d"), R=[MODb])
        S.finish()
        S.emit(nc)
        return nc

    def modrow(l, r, i):
        return MOD[l, r, i * 1024:(i + 1) * 1024]

    base_mark = A.mark()
    OML = [A.alloc(D, F32) for _ in range(2)]
    mt = A.mark()
    lbt = A.alloc(6 * D, F32)
    lv = lbt.ap.rearrange("p (a d) -> p a d", a=6)
    mx = A.alloc(D, F32)
    S.dma("sp", lbt.ap, hg_lb.rearrange("a b d -> (a b d)").partition_broadcast(128), W=[lbt.b])
    for d in range(2):
        S.tt("dve", mx.ap, lv[:, 3 * d, :], lv[:, 3 * d + 1, :], ALU.max, R=[lbt.b], W=[mx.b])
        S.tt("dve", mx.ap, mx.ap, lv[:, 3 * d + 2, :], ALU.max, R=[lbt.b, mx.b], W=[mx.b])
        for i in range(3):
            S.tt("dve", lv[:, 3 * d + i, :], lv[:, 3 * d + i, :], mx.ap, ALU.subtract, R=[lbt.b, mx.b], W=[lbt.b])
    S.act(lbt.ap, lbt.ap, AF.Exp, R=[lbt.b], W=[lbt.b])
    for d in range(2):
        S.tt("dve", OML[d].ap, lv[:, 3 * d + 1, :], lv[:, 3 * d + 2, :], ALU.add, R=[lbt.b], W=[OML[d].b])
        S.tt("dve", mx.ap, OML[d].ap, lv[:, 3 * d, :], ALU.add, R=[lbt.b, OML[d].b], W=[mx.b])
        S.add("dve", lambda e: e.reciprocal(mx.ap, mx.ap), R=[mx.b], W=[mx.b], small=True)
        S.tt("dve", OML[d].ap, OML[d].ap, mx.ap, ALU.mult, R=[OML[d].b, mx.b], W=[OML[d].b])
    A.reset(mt)
    S.barrier()

    m1 = A.mark()
    WIN = A.alloc(8 * 5 * D, BF16)
    WINv = WIN.ap.rearrange("p (k n) -> p k n", k=8)
    for k in range(8):
        S.dma("pool", WINv[:, k, :], hg_w_in[0, k * 128:(k + 1) * 128, :], W=[WIN.b])
    SC1 = [A.alloc(D, F32) for _ in range(2)]
    SH1 = [A.alloc(D, F32) for _ in range(2)]
    for r in range(2):
        load_row(g, SH1[r], modrow(0, r, 0), R=[MODb])
        load_row(g, SC1[r], modrow(0, r, 1), R=[MODb])
        S.ts("dve", SC1[r].ap, SC1[r].ap, 1.0, None, ALU.add, R=[SC1[r].b], W=[SC1[r].b])
    xb = [A.alloc(D, F32) for _ in range(2)]
    hb = A.alloc(D, F32)
    hT = A.alloc(D, BF16)
    qsb = A.alloc(D, F32)
    vsb = A.alloc(D, BF16)
    gsb = A.alloc(D, F32)
    zbsb = A.alloc(D, F32)
    ofsb = A.alloc(D, F32)
    alloc_hg_tmp(g)

    def evac1(hf, P_o, P_ox):
        hs = slice(hf * 512, (hf + 1) * 512)
        S.copy("act", ofsb.ap[:, hs], P_o.ap, R=[P_o.b], W=[ofsb.b])
        S.tt("dve", ofsb.ap[:, hs], ofsb.ap[:, hs], P_ox.ap, ALU.add, R=[ofsb.b, P_ox.b], W=[ofsb.b])

    chunk_order = [0, 1, 2, 3, 4, 5, 8, 9, 6, 7]
    for t in range(NT if stop != 'p2sim' else 0):
        r = 1 if t < 2 else 0
        xt = xb[t % 2]
        S.dma("sp", xt.ap, tok_src(t), W=[xt.b])
        S.tt("dve", hb.ap, xt.ap, SC1[r].ap, ALU.mult, R=[xt.b, SC1[r].b], W=[hb.b])
        S.tt("dve", hb.ap, hb.ap, SH1[r].ap, ALU.add, R=[hb.b, SH1[r].b], W=[hb.b])
        for gi in range(2):
            P = PS[gi]
            for j in range(4):
                k = gi * 4 + j
                S.tr(P.ap[:, j * 128:(j + 1) * 128], hb.ap[:, k * 128:(k + 1) * 128], g.ident, R=[hb.b, g.CST.b], W=[P.b])
            S.copy("act", hT.ap[:, gi * 512:(gi + 1) * 512], P.ap, R=[P.b], W=[hT.b])
        zP = {}
        for ci, cidx in enumerate(chunk_order):
            P = PS[2 + ci % 2]
            for k in range(8):
                S.mm(P.ap, hT.ap[:, k * 128:(k + 1) * 128], WINv[:, k, cidx * 512:(cidx + 1) * 512], k == 0, k == 7,
                     R=[hT.b, WIN.b], W=[P.b])
            typ, hf = divmod(cidx, 2)
            hs = slice(hf * 512, (hf + 1) * 512)
            if typ == 0:
                S.copy("act", qsb.ap[:, hs], P.ap, R=[P.b], W=[qsb.b])
            elif typ == 1:
                S.copy("act", vsb.ap[:, hs], P.ap, R=[P.b], W=[vsb.b])
            elif typ == 2:
                S.copy("act", gsb.ap[:, hs], P.ap, R=[P.b], W=[gsb.b])
            elif typ == 4:
                S.copy("act", zbsb.ap[:, hs], P.ap, R=[P.b], W=[zbsb.b])
            else:
                zP[hf] = P
        for hf in range(2):
            hg_half(g, 0, hf, zP[hf].ap, [zP[hf].b], qsb, vsb, OML[0], evac1)
            if stop == "p1dbg" and t == 0 and hf == 0:
                sg, kk, lf, eb, enb, er, qd, ki, ke, qdT, kiT, ATs, dec = g.Tm
                S.dma("sp", dbg[0:128, :], hb.ap, R=[hb.b])
                S.dma("sp", dbg[128:256, :], qsb.ap, R=[qsb.b])
                S.dma("sp", dbg[256:384, :], gsb.ap, R=[gsb.b])
                S.dma("sp", dbg[384:512, :], zbsb.ap, R=[zbsb.b])
                for i, tl in enumerate((sg, kk, lf, eb, enb, er)):
                    S.dma("sp", dbg[512 + 128 * (i // 2):640 + 128 * (i // 2), (i % 2) * 512:(i % 2) * 512 + 512], tl.ap, R=[tl.b])
                S.dma("sp", dbg[896:1024, :], ofsb.ap, R=[ofsb.b])
                S.dma("sp", dbg[1152:1280, :], OML[0].ap, R=[OML[0].b])
        rs = slice(t * 128, (t + 1) * 128)
        S.dma("sp", Qs[rs, :], qsb.ap, R=[qsb.b], W=[p1b[t]])
        S.dma("sp", Vs[rs, :], vsb.ap, R=[vsb.b], W=[p1b[t]])
        S.dma("sp", Gs[rs, :], gsb.ap, R=[gsb.b], W=[p1b[t]])
        S.dma("sp", ZBs[rs, :], zbsb.ap, R=[zbsb.b], W=[p1b[t]])
        S.dma("sp", OFs[rs, :], ofsb.ap, R=[ofsb.b], W=[p1b[t]])
    A.reset(m1)
    S.barrier()
    if stop == "p1dbg":
        S.finish()
        S.emit(nc)
        return nc
    if stop == "p1":
        for t in range(NT):
            S.dma("sp", dbg[t * 128:(t + 1) * 128, :], OFs[t * 128:(t + 1) * 128, :], R=[p1b[t]])
        S.finish()
        S.emit(nc)
        return nc

    m2 = A.mark()
    WOUT = A.alloc(8 * D, BF16)
    WOUTv = Tile(WOUT.ap.rearrange("p (k n) -> p k n", k=8))
    WOUTv.b = WOUT.b
    for k in range(8):
        S.dma("pool", WOUTv.ap[:, k, :], hg_w_out[0, k * 128:(k + 1) * 128, :], W=[WOUT.b])
    WR = A.alloc(8 * NE, F32)
    WRv = Tile(WR.ap.rearrange("p (k n) -> p k n", k=8))
    WRv.b = WR.b
    for k in range(8):
        S.dma("sp", WRv.ap[:, k, :], moe_w_router[0, k * 128:(k + 1) * 128, :], W=[WR.b])
    BR = A.alloc(NE, F32)
    load_row(g, BR, moe_b_router[0])
    G1 = [A.alloc(D, F32) for _ in range(2)]
    SC2 = [A.alloc(D, F32) for _ in range(2)]
    SH2 = [A.alloc(D, F32) for _ in range(2)]
    for r in range(2):
        load_row(g, G1[r], modrow(0, r, 2), R=[MODb])
        load_row(g, SH2[r], modrow(0, r, 3), R=[MODb])
        load_row(g, SC2[r], modrow(0, r, 4), R=[MODb])
        S.ts("dve", SC2[r].ap, SC2[r].ap, 1.0, None, ALU.add, R=[SC2[r].b], W=[SC2[r].b])
    LNG = A.alloc(D, F32)
    LNB = A.alloc(D, F32)
    NG = A.alloc(D, F32)
    load_row(g, LNG, ln_g[0, 0])
    load_row(g, LNB, ln_b[0, 0])
    load_row(g, NG, hg_norm_g[0])
    ld = [[A.alloc(D, F32) for _ in range(5)] + [A.alloc(D, BF16)] for _ in range(2)]
    osb = A.alloc(D, F32)
    sq = A.alloc(D, F32)
    ms = A.alloc(8, F32)
    ybf = A.alloc(D, BF16)
    alloc_hg_tmp(g)
    alloc_tail_tmp(g)

    order2 = [1, 0] + list(range(NT - 1, 1, -1))
    if stop == 'p2sim':
        order2 = [1, 65]
    for i, t in enumerate(order2):
        r = 1 if t < 2 else 0
        q2, zb2, g2, of2, x2, v2 = ld[i % 2]
        rs = slice(t * 128, (t + 1) * 128)
        S.dma("sp", q2.ap, Qs[rs, :], R=[p1b[t]], W=[q2.b])
        S.dma("sp", zb2.ap, ZBs[rs, :], R=[p1b[t]], W=[zb2.b])
        S.dma("sp", g2.ap, Gs[rs, :], R=[p1b[t]], W=[g2.b])
        S.dma("sp", of2.ap, OFs[rs, :], R=[p1b[t]], W=[of2.b])
        S.dma("sp", v2.ap, Vs[rs, :], R=[p1b[t]], W=[v2.b])
        S.dma("sp", x2.ap, tok_src(t), W=[x2.b])

        def evac2(hf, P_o, P_ox, of2=of2):
            hs = slice(hf * 512, (hf + 1) * 512)
            S.tt("dve", osb.ap[:, hs], P_o.ap, of2.ap[:, hs], ALU.add, R=[P_o.b, of2.b], W=[osb.b])
            S.tt("dve", osb.ap[:, hs], osb.ap[:, hs], P_ox.ap, ALU.add, R=[osb.b, P_ox.b], W=[osb.b])

        for hf in range(2):
            hs = slice(hf * 512, (hf + 1) * 512)
            hg_half(g, 1, hf, zb2.ap[:, hs], [zb2.b], q2, v2, OML[1], evac2)
            if g.cut == 5 and i == 0 and hf == 0:
                sg_, kk_, lf_, eb_, enb_, er_ = g.Tm[0:6]
                S.dma("sp", dbg[0:128, :], q2.ap, R=[q2.b])
                S.dma("sp", dbg[128:256, :], zb2.ap, R=[zb2.b])
                S.dma("sp", dbg[256:384, :], of2.ap, R=[of2.b])
                for ii, tl in enumerate((sg_, kk_, lf_, eb_, enb_, er_)):
                    S.dma("sp", dbg[512 + 128 * (ii // 2):640 + 128 * (ii // 2), (ii % 2) * 512:(ii % 2) * 512 + 512], tl.ap, R=[tl.b])
                S.dma("sp", dbg[896:1024, :], osb.ap, R=[osb.b])
                S.dma("sp", dbg[1152:1280, :], OML[1].ap, R=[OML[1].b])
                S.finish()
                S.emit(nc)
                return nc
        if g.cut == 1:
            S.dma("sp", dbg[rs, :], osb.ap, R=[osb.b])
            continue
        S.tt("pool", sq.ap, osb.ap, osb.ap, ALU.mult, R=[osb.b], W=[sq.b])
        S.add("dve", lambda e: e.tensor_reduce(ms.ap, sq.ap.rearrange("p (h d) -> p h d", h=8), AX.X, ALU.add), R=[sq.b], W=[ms.b], small=True)
        S.act(ms.ap, ms.ap, AF.Sqrt, R=[ms.b], W=[ms.b], bias=g.EPSRMS.ap[:, 0:1], scale=1.0 / 128.0)
        S.add("dve", lambda e: e.reciprocal(ms.ap, ms.ap), R=[ms.b], W=[ms.b], small=True)
        S.act(g2.ap, g2.ap, AF.Silu, R=[g2.b], W=[g2.b])
        for h in range(8):
            hc = slice(h * 128, (h + 1) * 128)
            S.ts("dve", sq.ap[:, hc], osb.ap[:, hc], ms.ap[:, h:h + 1], None, ALU.mult, R=[osb.b, ms.b], W=[sq.b], force=(h == 0))
        S.tt("pool", g2.ap, g2.ap, NG.ap, ALU.mult, R=[g2.b, NG.b], W=[g2.b])
        S.tt("dve", ybf.ap, sq.ap, g2.ap, ALU.mult, R=[sq.b, g2.b], W=[ybf.b])
        if g.cut == 2:
            S.dma("sp", dbg[rs, :], osb.ap, R=[osb.b])
            continue
        g.dbg_extra = (ms, osb, g2, sq)
        mixer_tail(g, t, ybf, x2, r, (G1, LNG, LNB, SC2, SH2), WOUTv, WRv, BR, X1, H2T, GATES)
        if getattr(g, "early", False):
            S.finish()
            S.emit(nc)
            return nc
    if g.cut in (1, 2):
        S.finish()
        S.emit(nc)
        return nc
    A.reset(m2)
    S.barrier()

    if stop in ("l0mix", "p2sim"):
        for t in (range(NT) if stop == "l0mix" else order2):
            S.dma("sp", dbg[t * 128:(t + 1) * 128, :], X1[t * 128:(t + 1) * 128, :], R=[g.X1b[t]])
        S.finish()
        S.emit(nc)
        return nc

    A.reset(base_mark)
    X2 = dsc("X2", [T_ALL, D])
    X2b = [Buf() for _ in range(NT)]
    mdr = (moe_w_gu, moe_b_gu, moe_w_dn, moe_b_dn, ln_g, ln_b, H2T, GATES)
    moe_tiles = list(range(NT))
    if g.cut == 11:
        moe_tiles = list(range(11))
    moe_phase(g, 0, moe_tiles, mdr, X1, g.X1b, lambda t: X2[t * 128:(t + 1) * 128, :], lambda t: X2b[t], MODb, modrow)
    if stop == "l0moe":
        for t in moe_tiles:
            S.dma("sp", dbg[t * 128:(t + 1) * 128, :], X2[t * 128:(t + 1) * 128, :], R=[X2b[t]])
            if len(moe_tiles) <= 11:
                S.dma("sp", dbg[(20 + t) * 128:(21 + t) * 128, 0:32], GATES[t * 128:(t + 1) * 128, :], R=[g.GATESb[t]])
        S.finish()
        S.emit(nc)
        return nc

    A.reset(base_mark)
    QT = dsc("QT", [16, 128, T_LAT], BF16)
    KT = dsc("KT", [16, 128, T_ALL], BF16)
    VB = dsc("VB", [T_ALL, D], BF16)
    YT = dsc("YT", [8, 128, T_LAT], BF16)
    qkvb = Buf()
    YTb = Buf()
    attn_prep(g, (da_w_in, rope), X2, X2b, QT, KT, VB, qkvb, MODb, modrow)
    attn_phase(g, (da_lam, da_norm_g), QT, KT, VB, YT, qkvb, YTb)
    if stop == "l1attn":
        for h in range(8):
            S.dma("sp", dbg[h * 128:(h + 1) * 128, :], YT[h, :, 0:1024], R=[YTb])
        S.finish()
        S.emit(nc)
        return nc

    m3 = A.mark()
    X3 = dsc("X3", [T_ALL, D])
    X3b = [Buf() for _ in range(NT)]
    g.X1b = X3b
    WOUT = A.alloc(8 * D, BF16)
    WOUTv = Tile(WOUT.ap.rearrange("p (k n) -> p k n", k=8))
    WOUTv.b = WOUT.b
    for k in range(8):
        S.dma("pool", WOUTv.ap[:, k, :], da_w_out[0, k * 128:(k + 1) * 128, :], W=[WOUT.b])
    WR = A.alloc(8 * NE, F32)
    WRv = Tile(WR.ap.rearrange("p (k n) -> p k n", k=8))
    WRv.b = WR.b
    for k in range(8):
        S.dma("sp", WRv.ap[:, k, :], moe_w_router[1, k * 128:(k + 1) * 128, :], W=[WR.b])
    BR = A.alloc(NE, F32)
    load_row(g, BR, moe_b_router[1])
    G1 = [A.alloc(D, F32)]
    SC2 = [A.alloc(D, F32)]
    SH2 = [A.alloc(D, F32)]
    load_row(g, G1[0], modrow(1, 0, 2), R=[MODb])
    load_row(g, SH2[0], modrow(1, 0, 3), R=[MODb])
    load_row(g, SC2[0], modrow(1, 0, 4), R=[MODb])
    S.ts("dve", SC2[0].ap, SC2[0].ap, 1.0, None, ALU.add, R=[SC2[0].b], W=[SC2[0].b])
    LNG = A.alloc(D, F32)
    LNB = A.alloc(D, F32)
    load_row(g, LNG, ln_g[1, 0])
    load_row(g, LNB, ln_b[1, 0])
    yTl = [A.alloc(D, BF16) for _ in range(2)]
    x2l = [A.alloc(D, F32) for _ in range(2)]
    alloc_tail_tmp(g)
    for t in range(2, NT):
        yt_ = yTl[t % 2]
        xt_ = x2l[t % 2]
        tok0 = (t - 2) * 128
        S.dma("sp", yt_.ap.rearrange("p (k n) -> p k n", k=8), YT[:, :, tok0:tok0 + 128].rearrange("k p n -> p k n"), R=[YTb], W=[yt_.b])
        S.dma("sp", xt_.ap, X2[t * 128:(t + 1) * 128, :], R=[X2b[t]], W=[xt_.b])
        mixer_tail(g, t, None, xt_, 0, (G1, LNG, LNB, SC2, SH2), WOUTv, WRv, BR, X3, H2T, GATES, yT_in=yt_)
    A.reset(m3)
    S.barrier()
    if stop == "l1mix":
        for t in range(2, NT):
            S.dma("sp", dbg[t * 128:(t + 1) * 128, :], X3[t * 128:(t + 1) * 128, :], R=[X3b[t]])
        S.finish()
        S.emit(nc)
        return nc

    A.reset(base_mark)
    outb = [Buf() for _ in range(NT)]
    moe_phase(g, 1, list(range(2, NT)), mdr, X3, X3b, lambda t: out[(t - 2) * 128:(t - 1) * 128, :], lambda t: outb[t], MODb, modrow)
    S.finish()
    S.emit(nc)
    return nc


def make_in_maps(inputs, names=None):
    cst = make_consts()
    rope = make_rope()
    shared = {k: np.ascontiguousarray(inputs[k]) for k in (
        "w_ada", "b_ada", "ln_g", "ln_b", "hg_w_in", "hg_lb", "hg_norm_g", "hg_w_out", "da_w_in", "da_lam",
        "da_norm_g", "da_w_out", "moe_w_router", "moe_b_router", "moe_w_gu", "moe_b_gu", "moe_w_dn", "moe_b_dn")}
    shared["cst"] = cst
    shared["rope"] = rope
    maps = []
    for b in range(N_CORES):
        m = dict(shared)
        m["x"] = np.ascontiguousarray(inputs["x"][b])
        m["ctx"] = np.ascontiguousarray(inputs["ctx"][b])
        m["c2"] = np.ascontiguousarray(np.stack([inputs["c"][b], inputs["c_ctx"]], axis=0))
        if names is not None:
            m = {k: v for k, v in m.items() if k in names}
        maps.append(m)
    return maps


def kernel(**inputs):
    nc = build()
    maps = make_in_maps(inputs)
    res = run_bass_kernel_spmd(nc, maps, core_ids=list(range(N_CORES)))
    return np.stack([np.asarray(r["out"], dtype=np.float32) for r in res.results], axis=0)


def moe_phase(g, l, tiles, dr, X1, X1b, dst_of, dst_b_of, MODb, modrow):
    S, A, PS = g.S, g.A, g.PS
    mk = A.mark()
    w_gu, b_gu, w_dn, b_dn, ln_g, ln_b, H2T, GATES = dr
    GMAX = 11
    BG32 = A.alloc(2048, F32, parts=32)
    S.dma("sp", BG32.ap, b_gu[l], W=[BG32.b])
    BGUT = A.alloc(512, F32)
    Pt = PS[0]
    for fc in range(16):
        S.tr(Pt.ap[:, fc * 32:(fc + 1) * 32], BG32.ap[:, fc * 128:(fc + 1) * 128], g.CST.ap[0:32, 0:32], R=[BG32.b, g.CST.b], W=[Pt.b])
    S.copy("dve", BGUT.ap, Pt.ap, R=[Pt.b], W=[BGUT.b])
    BDN = A.alloc(1024, F32, parts=32)
    S.dma("sp", BDN.ap, b_dn[l], W=[BDN.b])
    G2 = [A.alloc(D, F32) for _ in range(2)]
    for r in range(2):
        load_row(g, G2[r], modrow(l, r, 5), R=[MODb])
    LNG = A.alloc(D, F32)
    LNB = A.alloc(D, F32)
    load_row(g, LNG, ln_g[l, 1])
    load_row(g, LNB, ln_b[l, 1])
    h2T = A.alloc(GMAX * 1024, BF16)
    h2Tv = h2T.ap.rearrange("p (t k n) -> p t k n", t=GMAX, k=8)
    gt = A.alloc(GMAX * NE, F32)
    GT = A.alloc(GMAX * 128, F32, parts=32)
    acc = A.alloc(GMAX * 1024, F32)
    accb = [Buf() for _ in range(GMAX)]
    WG = [A.alloc(8 * 2 * 512, BF16) for _ in range(2)]
    WD = [A.alloc(4 * 1024, BF16) for _ in range(2)]
    gc = [A.alloc(512, F32) for _ in range(2)]
    sg = [A.alloc(512, F32) for _ in range(2)]
    u0 = [A.alloc(512, F32) for _ in range(2)]
    tg = [A.alloc(512, F32) for _ in range(2)]
    actT = [A.alloc(4 * 512, BF16) for _ in range(2)]
    ysc = [A.alloc(512, F32) for _ in range(2)]
    xl = [A.alloc(D, F32) for _ in range(2)]
    rr = A.alloc(D, F32)
    xo = A.alloc(D, F32)
    g.ln_st = A.alloc(12, F32)
    g.ln_mv = A.alloc(2, F32)
    g.ln_rstd = A.alloc(1, F32)

    groups = [tiles[i:i + GMAX] for i in range(0, len(tiles), GMAX)]
    wcount = [0]

    def load_w(e, hx, b):
        wg = WG[b].ap.rearrange("p (k c f) -> p k c f", k=8, c=2)
        for c in range(2):
            S.dma("pool", wg[:, :, c, :],
                  w_gu[l, e, :, c * 1024 + hx * 512:c * 1024 + hx * 512 + 512].rearrange("(k p) f -> p k f", p=128), W=[WG[b].b])
        wd = WD[b].ap.rearrange("p (j n) -> p j n", j=4)
        S.dma("pool", wd, w_dn[l, e, hx * 512:(hx + 1) * 512, :].rearrange("(j p) n -> p j n", p=128), W=[WD[b].b])

    for grp in groups:
        G_ = len(grp)
        for ti, t in enumerate(grp):
            S.dma("sp", h2T.ap[:, ti * 1024:(ti + 1) * 1024], H2T[t].rearrange("p k n -> p (k n)"), R=[g.H2Tb[t]], W=[h2T.b])
            S.dma("sp", gt.ap[:, ti * NE:(ti + 1) * NE], GATES[t * 128:(t + 1) * 128, :], R=[g.GATESb[t]], W=[gt.b])
        seq = [(e, hx) for e in range(NE) for hx in range(2)]
        load_w(seq[0][0], seq[0][1], 0)
        for ti in range(G_):
            Pq = PS[1]
            S.tr(Pq.ap[0:32, 0:128], gt.ap[:, ti * NE:(ti + 1) * NE], g.ident, R=[gt.b, g.CST.b], W=[Pq.b])
            S.copy("dve", GT.ap[:, ti * 128:(ti + 1) * 128], Pq.ap[0:32, 0:128], R=[Pq.b], W=[GT.b])
            for n in range(2):
                Py = PS[2 + n]
                S.mm(Py.ap, GT.ap[:, ti * 128:(ti + 1) * 128], BDN.ap[:, n * 512:(n + 1) * 512], True, True, R=[GT.b, BDN.b], W=[Py.b])
                S.copy("act", acc.ap[:, ti * 1024 + n * 512:ti * 1024 + (n + 1) * 512], Py.ap, R=[Py.b], W=[accb[ti]])
        chunks = [(c0, min(4, G_ - c0)) for c0 in range(0, G_, 4)]
        for si, (e, hx) in enumerate(seq):
            b = si % 2
            if si + 1 < len(seq):
                load_w(seq[si + 1][0], seq[si + 1][1], 1 - b)
            wg = WG[b].ap.rearrange("p (k c f) -> p k c f", k=8, c=2)
            wd = WD[b].ap.rearrange("p (j n) -> p j n", j=4)
            for ci, (c0, nt) in enumerate(chunks):
                N = nt * 128
                aT = actT[ci % 2]
                aTv = aT.ap.rearrange("p (j n) -> p j n", j=4)
                for j in range(4):
                    pb = j % 2
                    Pg, Pu = PS[pb * 2], PS[pb * 2 + 1]
                    for c, P in ((0, Pg), (1, Pu)):
                        for k in range(8):
                            S.mm(P.ap[:, 0:N], wg[:, k, c, j * 128:(j + 1) * 128], h2Tv[:, c0:c0 + nt, k, :], k == 0, k == 7,
                                 R=[WG[b].b, h2T.b], W=[P.b])
                    fc = hx * 4 + j
                    bg = BGUT.ap[:, fc * 32 + e:fc * 32 + e + 1]
                    bu = BGUT.ap[:, (8 + fc) * 32 + e:(8 + fc) * 32 + e + 1]
                    S.ts("dve", gc[pb].ap[:, 0:N], Pg.ap[:, 0:N], bg, 7.0, ALU.add, ALU.min, R=[Pg.b, BGUT.b], W=[gc[pb].b])
                    S.act(sg[pb].ap[:, 0:N], gc[pb].ap[:, 0:N], AF.Sigmoid, R=[gc[pb].b], W=[sg[pb].b], scale=1.702)
                    S.act(u0[pb].ap[:, 0:N], Pu.ap[:, 0:N], AF.Identity, R=[Pu.b, BGUT.b], W=[u0[pb].b], bias=bu)
                    S.ts("dve", u0[pb].ap[:, 0:N], u0[pb].ap[:, 0:N], 7.0, -7.0, ALU.min, ALU.max, R=[u0[pb].b], W=[u0[pb].b])
                    S.tt("pool", tg[pb].ap[:, 0:N], gc[pb].ap[:, 0:N], sg[pb].ap[:, 0:N], ALU.mult, R=[gc[pb].b, sg[pb].b], W=[tg[pb].b])
                    S.stt("dve", aTv[:, j, 0:N], u0[pb].ap[:, 0:N], 1.0, tg[pb].ap[:, 0:N], ALU.add, ALU.mult,
                          R=[u0[pb].b, tg[pb].b], W=[aT.b])
                for tl in range(nt):
                    ti = c0 + tl
                    for n in range(2):
                        Py = PS[4 + (tl * 2 + n) % 4]
                        for j in range(4):
                            S.mm(Py.ap, aTv[:, j, tl * 128:(tl + 1) * 128], wd[:, j, n * 512:(n + 1) * 512], j == 0, j == 3,
                                 R=[aT.b, WD[b].b], W=[Py.b])
                        asl = acc.ap[:, ti * 1024 + n * 512:ti * 1024 + (n + 1) * 512]
                        yb = ysc[(tl * 2 + n) % 2]
                        S.act(yb.ap, Py.ap, AF.Identity, R=[Py.b, gt.b], W=[yb.b], scale=gt.ap[:, ti * NE + e:ti * NE + e + 1])
                        S.tt("dve", asl, asl, yb.ap, ALU.add, R=[yb.b, accb[ti]], W=[accb[ti]])
        for ti, t in enumerate(grp):
            r = 1 if t < 2 else 0
            xt = xl[ti % 2]
            S.dma("sp", xt.ap, X1[t * 128:(t + 1) * 128, :], R=[X1b[t]], W=[xt.b])
            S.tt("dve", xo.ap, acc.ap[:, ti * 1024:(ti + 1) * 1024], G2[r].ap, ALU.mult, R=[accb[ti], G2[r].b], W=[xo.b])
            S.stt("dve", rr.ap, xt.ap, ALPHA, xo.ap, ALU.mult, ALU.add, R=[xt.b, xo.b], W=[rr.b])
            layer_norm_rows(g, rr, LNG, LNB, xo)
            S.dma("sp", dst_of(t), xo.ap, R=[xo.b], W=[dst_b_of(t)])
    A.reset(mk)
    S.barrier()


def attn_prep(g, dr, X2, X2b, QT, KT, VB, qkvb, MODb, modrow):
    S, A, PS = g.S, g.A, g.PS
    da_w_in, rope = dr
    mk = A.mark()
    DAW = A.alloc(8 * 3 * D, BF16)
    DAWv = DAW.ap.rearrange("p (k n) -> p k n", k=8)
    for k in range(8):
        S.dma("pool", DAWv[:, k, :], da_w_in[0, k * 128:(k + 1) * 128, :], W=[DAW.b])
    SC1 = [A.alloc(D, F32) for _ in range(2)]
    SH1 = [A.alloc(D, F32) for _ in range(2)]
    for r in range(2):
        load_row(g, SH1[r], modrow(1, r, 0), R=[MODb])
        load_row(g, SC1[r], modrow(1, r, 1), R=[MODb])
        S.ts("dve", SC1[r].ap, SC1[r].ap, 1.0, None, ALU.add, R=[SC1[r].b], W=[SC1[r].b])
    xb = [A.alloc(D, F32) for _ in range(2)]
    rp = [A.alloc(D, F32) for _ in range(2)]
    hb = A.alloc(D, F32)
    hT = A.alloc(D, BF16)
    qr = A.alloc(512, F32)
    t2 = A.alloc(512, F32)
    sq = A.alloc(512, F32)
    n2 = A.alloc(8, F32)
    qa = A.alloc(8 * 128, BF16)
    ka = A.alloc(8 * 128, BF16)
    qav = qa.ap.rearrange("p (h d) -> p h d", h=8)
    kav = ka.ap.rearrange("p (h d) -> p h d", h=8)
    S.memset("dve", qa.ap, 0.0, W=[qa.b])
    S.memset("dve", ka.ap, 0.0, W=[ka.b])
    aT = [A.alloc(8 * 128, BF16) for _ in range(2)]
    vsb = A.alloc(D, BF16)
    kmx = A.alloc(16, F32)
    S.memset("dve", kmx.ap, 0.0, W=[kmx.b])
    g.kmx = kmx

    def rope_chunk(P, rpt, ch, do_rope):
        if not do_rope:
            S.copy("act", qr.ap, P.ap, R=[P.b], W=[qr.b])
            return
        C8 = rpt.ap[:, 0:512]
        S8 = rpt.ap[:, 512:1024]
        S.tt("dve", qr.ap, P.ap, C8, ALU.mult, R=[P.b, rpt.b], W=[qr.b])
        Pv = P.ap.rearrange("p (a b d) -> p a b d", b=2, d=16)
        t2v = t2.ap.rearrange("p (a b d) -> p a b d", b=2, d=16)
        S8v = S8.rearrange("p (a b d) -> p a b d", b=2, d=16)
        for ab in range(2):
            S.tt("dve", t2v[:, :, ab, :], Pv[:, :, 1 - ab, :], S8v[:, :, ab, :], ALU.mult, R=[P.b, rpt.b], W=[t2.b])
        S.tt("pool", qr.ap, qr.ap, t2.ap, ALU.add, R=[qr.b, t2.b], W=[qr.b])

    for t in range(NT):
        r = 1 if t < 2 else 0
        lat = t >= 2
        xt = xb[t % 2]
        rpt = rp[t % 2]
        S.dma("sp", xt.ap, X2[t * 128:(t + 1) * 128, :], R=[X2b[t]], W=[xt.b])
        if lat:
            S.dma("sp", rpt.ap, rope[(t - 2) * 128:(t - 1) * 128, :], W=[rpt.b])
        S.tt("dve", hb.ap, xt.ap, SC1[r].ap, ALU.mult, R=[xt.b, SC1[r].b], W=[hb.b])
        S.tt("dve", hb.ap, hb.ap, SH1[r].ap, ALU.add, R=[hb.b, SH1[r].b], W=[hb.b])
        for gi in range(2):
            P = PS[gi]
            for j in range(4):
                k = gi * 4 + j
                S.tr(P.ap[:, j * 128:(j + 1) * 128], hb.ap[:, k * 128:(k + 1) * 128], g.ident, R=[hb.b, g.CST.b], W=[P.b])
            S.copy("act", hT.ap[:, gi * 512:(gi + 1) * 512], P.ap, R=[P.b], W=[hT.b])
        chunks = ([0, 1] if lat else []) + [2, 3, 4, 5]
        for ci, cidx in enumerate(chunks):
            P = PS[2 + ci % 2]
            for k in range(8):
                S.mm(P.ap, hT.ap[:, k * 128:(k + 1) * 128], DAWv[:, k, cidx * 512:(cidx + 1) * 512], k == 0, k == 7,
                     R=[hT.b, DAW.b], W=[P.b])
            typ, hf = divmod(cidx, 2)
            if typ == 2:
                S.copy("act", vsb.ap[:, hf * 512:(hf + 1) * 512], P.ap, R=[P.b], W=[vsb.b])
                continue
            rope_chunk(P, rpt, hf, lat)
            S.tt("pool", sq.ap, qr.ap, qr.ap, ALU.mult, R=[qr.b], W=[sq.b])
            S.add("dve", lambda e: e.tensor_reduce(n2.ap, sq.ap.rearrange("p (h d) -> p h d", h=8), AX.X, ALU.add), R=[sq.b], W=[n2.b], small=True)
            qrv = qr.ap.rearrange("p (h d) -> p h d", h=8)
            if typ == 0:
                av, at_ = qav, qa
                S.act(av[:, :, 0:64], qrv, AF.Identity, R=[qr.b], W=[at_.b], scale=0.125)
                S.act(n2.ap, n2.ap, AF.Sqrt, R=[n2.b], W=[n2.b], scale=1.0 / 64.0)
                S.ts("dve", av[:, :, 64], n2.ap, -1.0, None, ALU.mult, R=[n2.b], W=[at_.b])
                dstT = QT
                tok0 = (t - 2) * 128
            else:
                av, at_ = kav, ka
                S.copy("act", av[:, :, 0:64], qrv, R=[qr.b], W=[at_.b])
                S.tt("dve", kmx.ap[:, hf * 8:(hf + 1) * 8], kmx.ap[:, hf * 8:(hf + 1) * 8], n2.ap, ALU.max, R=[kmx.b, n2.b], W=[kmx.b])
                dstT = KT
                tok0 = t * 128
            Pt = PS[4 + ci % 2]
            Ptv = Pt.ap.bitcast(BF16)
            for hm in range(8):
                S.tr(Ptv[:, hm * 128:(hm + 1) * 128], av[:, hm, :], g.IDB.ap, R=[at_.b, g.IDB.b], W=[Pt.b])
            a_t = aT[ci % 2]
            S.copy("act", a_t.ap, Ptv, R=[Pt.b], W=[a_t.b])
            S.dma("sp", dstT[hf * 8:(hf + 1) * 8, :, tok0:tok0 + 128].rearrange("h p n -> p h n"),
                  a_t.ap.rearrange("p (h n) -> p h n", h=8), R=[a_t.b], W=[qkvb])
        S.dma("sp", VB[t * 128:(t + 1) * 128, :], vsb.ap, R=[vsb.b], W=[qkvb])
    A.reset(mk)
    S.barrier()


def attn_phase(g, dr, QT, KT, VB, YT, qkvb, YTb):
    S, A, PS = g.S, g.A, g.PS
    da_lam, da_norm_g = dr
    mk = A.mark()
    kmx = g.kmx
    Pk = PS[0]
    S.tr(Pk.ap[0:16, 0:128], kmx.ap, g.ident, R=[kmx.b, g.CST.b], W=[Pk.b])
    kv = A.alloc(1, F32, parts=16)
    S.add("dve", lambda e: e.tensor_reduce(kv.ap, Pk.ap[0:16, 0:128], AX.X, ALU.max), R=[Pk.b], W=[kv.b], small=True)
    S.act(kv.ap, kv.ap, AF.Sqrt, R=[kv.b], W=[kv.b])
    dg = A.alloc(16, F32, parts=16)
    S.ts("dve", dg.ap, g.CST.ap[0:16, 0:16], kv.ap[:, 0:1], None, ALU.mult, R=[g.CST.b, kv.b], W=[dg.b])
    on16 = A.alloc(128, F32, parts=16)
    S.memset("dve", on16.ap, 1.0, W=[on16.b])
    Pk2 = PS[1]
    S.mm(Pk2.ap[:, 0:16], on16.ap, dg.ap, True, True, R=[on16.b, dg.b], W=[Pk2.b])
    KMB = A.alloc(16, F32)
    S.copy("dve", KMB.ap, Pk2.ap[:, 0:16], R=[Pk2.b], W=[KMB.b])
    lp = A.alloc(256, F32)
    S.dma("sp", lp.ap, da_lam[0].rearrange("a d -> (a d)").partition_broadcast(128), W=[lp.b])
    pr = A.alloc(128, F32)
    lpv = lp.ap.rearrange("p (a b d) -> p a b d", a=2, b=2)
    prv = pr.ap.rearrange("p (a d) -> p a d", a=2)
    S.tt("dve", prv, lpv[:, :, 0, :], lpv[:, :, 1, :], ALU.mult, R=[lp.b], W=[pr.b])
    ls = A.alloc(2, F32)
    S.add("dve", lambda e: e.tensor_reduce(ls.ap, prv, AX.X, ALU.add), R=[pr.b], W=[ls.b], small=True)
    S.act(ls.ap, ls.ap, AF.Exp, R=[ls.b], W=[ls.b])
    NLAM = A.alloc(1, F32)
    S.tt("dve", NLAM.ap, ls.ap[:, 1:2], ls.ap[:, 0:1], ALU.subtract, R=[ls.b], W=[NLAM.b])
    S.ts("dve", NLAM.ap, NLAM.ap, -LAM_INIT1, None, ALU.add, R=[NLAM.b], W=[NLAM.b])
    NG1 = A.alloc(1, F32)
    S.dma("sp", NG1.ap, da_norm_g[0].rearrange("(p o) -> p o", o=1), W=[NG1.b])
    S.ts("dve", NG1.ap, NG1.ap, 1.0 - LAM_INIT1, None, ALU.mult, R=[NG1.b], W=[NG1.b])
    onc = A.alloc(1, F32)
    S.memset("dve", onc.ap, 1.0, W=[onc.b])
    onr = A.alloc(128, F32, parts=1)
    S.memset("dve", onr.ap, 1.0, W=[onr.b])

    Vh = A.alloc(NT * 128, BF16)
    kt = [A.alloc(T_ALL, BF16) for _ in range(2)]
    qt = [A.alloc(512, BF16) for _ in range(2)]
    pT = [A.alloc(512, BF16) for _ in range(3)]
    ZP = [A.alloc(512, F32) for _ in range(2)]
    ZD = [A.alloc(512, F32) for _ in range(2)]
    Bc = [A.alloc(512, F32) for _ in range(2)]
    rz = [A.alloc(512, F32, parts=1) for _ in range(2)]
    oT = A.alloc(512, F32)
    tmp = A.alloc(512, F32)
    yT = [A.alloc(512, BF16) for _ in range(2)]
    pcount = 0
    for h in range(8):
        S.dma("sp", Vh.ap.rearrange("p (t d) -> p t d", d=128), VB[:, h * 128:(h + 1) * 128].rearrange("(t p) d -> p t d", p=128),
              R=[qkvb], W=[Vh.b])
        for c in range(2):
            hm = 2 * h + c
            S.dma("sp", kt[c].ap, KT[hm], R=[qkvb], W=[kt[c].b])
            S.act(kt[c].ap[64:65, :], kt[c].ap[64:65, :], AF.Identity, R=[kt[c].b, KMB.b], W=[kt[c].b], bias=KMB.ap[64:65, hm:hm + 1])
        for qg in range(T_LAT // 512):
            for c in range(2):
                hm = 2 * h + c
                S.dma("sp", qt[c].ap, QT[hm, :, qg * 512:(qg + 1) * 512], R=[qkvb], W=[qt[c].b])
                Po = PS[2 + c]
                for ki in range(NT):
                    Psc = PS[ki % 2]
                    S.mm(Psc.ap, kt[c].ap[:, ki * 128:(ki + 1) * 128], qt[c].ap, True, True, R=[kt[c].b, qt[c].b], W=[Psc.b])
                    p = pT[pcount % 3]
                    pcount += 1
                    S.act(p.ap, Psc.ap, AF.Exp, R=[Psc.b], W=[p.b])
                    S.mm(Po.ap, Vh.ap[:, ki * 128:(ki + 1) * 128], p.ap, ki == 0, ki == NT - 1, R=[Vh.b, p.b], W=[Po.b])
                    if ki % 2 == 0:
                        eng, Z = "pool", ZP[c]
                    else:
                        eng, Z = "dve", ZD[c]
                    if ki < 2:
                        S.copy(eng, Z.ap, p.ap, R=[p.b], W=[Z.b])
                    else:
                        S.tt(eng, Z.ap, Z.ap, p.ap, ALU.add, R=[Z.b, p.b], W=[Z.b])
                Pz = PS[4 + c]
                S.mm(Pz.ap[0:1, :], onc.ap, ZP[c].ap, True, False, R=[onc.b, ZP[c].b], W=[Pz.b])
                S.mm(Pz.ap[0:1, :], onc.ap, ZD[c].ap, False, True, R=[onc.b, ZD[c].b], W=[Pz.b])
                S.add("dve", lambda e, c=c, Pz=Pz: e.reciprocal(rz[c].ap, Pz.ap[0:1, :]), R=[Pz.b], W=[rz[c].b], small=True)
                if c == 1:
                    S.ts("dve", rz[c].ap, rz[c].ap, NLAM.ap[0:1, 0:1], None, ALU.mult, R=[rz[c].b, NLAM.b], W=[rz[c].b])
                Pb = PS[6 + c]
                S.mm(Pb.ap, onr.ap, rz[c].ap, True, True, R=[onr.b, rz[c].b], W=[Pb.b])
                S.copy("act", Bc[c].ap, Pb.ap, R=[Pb.b], W=[Bc[c].b])
            S.tt("dve", oT.ap, PS[2].ap, Bc[0].ap, ALU.mult, R=[PS[2].b, Bc[0].b], W=[oT.b])
            S.tt("dve", tmp.ap, PS[3].ap, Bc[1].ap, ALU.mult, R=[PS[3].b, Bc[1].b], W=[tmp.b])
            S.tt("pool", oT.ap, oT.ap, tmp.ap, ALU.add, R=[oT.b, tmp.b], W=[oT.b])
            S.tt("pool", tmp.ap, oT.ap, oT.ap, ALU.mult, R=[oT.b], W=[tmp.b])
            Pm = PS[4]
            S.mm(Pm.ap[0:1, :], onc.ap, tmp.ap, True, True, R=[onc.b, tmp.b], W=[Pm.b])
            S.act(rz[0].ap, Pm.ap[0:1, :], AF.Sqrt, R=[Pm.b, g.EPSRMS.b], W=[rz[0].b], bias=g.EPSRMS.ap[0:1, 0:1], scale=1.0 / 128.0)
            S.add("dve", lambda e: e.reciprocal(rz[0].ap, rz[0].ap), R=[rz[0].b], W=[rz[0].b], small=True)
            Pb = PS[6]
            S.mm(Pb.ap, onr.ap, rz[0].ap, True, True, R=[onr.b, rz[0].b], W=[Pb.b])
            y = yT[qg % 2]
            S.stt("dve", y.ap, oT.ap, NG1.ap[:, 0:1], Pb.ap, ALU.mult, ALU.mult, R=[oT.b, NG1.b, Pb.b], W=[y.b])
            S.dma("sp", YT[h, :, qg * 512:(qg + 1) * 512], y.ap, R=[y.b], W=[YTb])
    A.reset(mk)
    S.barrier()
```
